# Optimizing a Trainium2 kernel written in Bass

```python
import jax, jax.numpy as jnp
from jax import lax
import numpy as np

D_MODEL = 2048
BATCH = 8
SEQ = 2048
DEPTH = 2

CTX_LEN = 256
GRID_W = 64
HEAD_DIM = 128
N_Q_HEADS = D_MODEL // HEAD_DIM
N_KV_HEADS = N_Q_HEADS // 4
GQA_GROUP = N_Q_HEADS // N_KV_HEADS
D_Q = N_Q_HEADS * HEAD_DIM
D_KV = N_KV_HEADS * HEAD_DIM
D_CONV = D_MODEL
CONV_WIDTH = 3
N_EXPERTS = 32
TOP_K = 4
D_EXPERT = D_MODEL // 2
SWIGLU_ALPHA = 1.702
SWIGLU_LIMIT = 7.0
ROPE_THETA = 10000.0
ROPE_AXIS_DIM = HEAD_DIM // 2
Q_BLOCK = 128
EXPERT_BLOCK = 128
NORM_EPS = 1e-6
DEEPNORM_ALPHA = (2 * DEPTH) ** 0.25
DEEPNORM_BETA = (8 * DEPTH) ** -0.25

O_Q = 0
O_K = O_Q + D_Q
O_V = O_K + D_KV
O_CB = O_V + D_KV
O_CC = O_CB + D_CONV
O_CX = O_CC + D_CONV
O_GA = O_CX + D_CONV
O_GC = O_GA + D_MODEL
N_IN = O_GC + D_MODEL

kernel_name = "hybrid_gqa_shortconv_moe_dit_block"


def layer_norm(x, g, b):
    xf = x.astype(jnp.float32)
    mu = jnp.mean(xf, axis=-1, keepdims=True)
    var = jnp.mean(jnp.square(xf - mu), axis=-1, keepdims=True)
    y = (xf - mu) * lax.rsqrt(var + NORM_EPS)
    return (y * g.astype(jnp.float32) + b.astype(jnp.float32)).astype(x.dtype)


def rms_norm(x, g):
    xf = x.astype(jnp.float32)
    y = xf * lax.rsqrt(jnp.mean(jnp.square(xf), axis=-1, keepdims=True) + NORM_EPS)
    return (y * g.astype(jnp.float32)).astype(x.dtype)


def axial_rope_tables(n_tok):
    rows = n_tok // GRID_W
    row = jnp.repeat(jnp.arange(rows, dtype=jnp.int32), GRID_W).astype(jnp.float32)
    col = jnp.tile(jnp.arange(GRID_W, dtype=jnp.int32), rows).astype(jnp.float32)
    inv = ROPE_THETA ** (-jnp.arange(0, ROPE_AXIS_DIM, 2, dtype=jnp.float32) / ROPE_AXIS_DIM)
    ang = jnp.concatenate([row[:, None] * inv, col[:, None] * inv], axis=-1)
    return jnp.cos(ang), jnp.sin(ang)


def apply_rope(x, cos, sin):
    xf = x.astype(jnp.float32).reshape(*x.shape[:-1], HEAD_DIM // 2, 2)
    x0, x1 = xf[..., 0], xf[..., 1]
    c = cos[None, :, None, :]
    s = sin[None, :, None, :]
    out = jnp.stack([x0 * c - x1 * s, x0 * s + x1 * c], axis=-1).reshape(x.shape)
    return out.astype(x.dtype)


def attend(qb, k, v):
    s = jnp.einsum('bqkgd,bskd->bkgqs', qb, k, preferred_element_type=jnp.float32)
    p = jax.nn.softmax(s, axis=-1)
    return jnp.einsum('bkgqs,bskd->bqkgd', p.astype(v.dtype), v)


def latent_attention(q, k_all, v_all):
    B, S = q.shape[0], q.shape[1]
    n_blk = S // Q_BLOCK
    qb = q.reshape(B, n_blk, Q_BLOCK, N_KV_HEADS, GQA_GROUP, HEAD_DIM).transpose(1, 0, 2, 3, 4, 5)
    o = lax.map(lambda qi: attend(qi, k_all, v_all), qb)
    return o.transpose(1, 0, 2, 3, 4, 5).reshape(B, S, D_Q)


def short_conv_mixer(gb, gc, xin, conv_w):
    z = gc * xin
    zp = jnp.pad(z, ((0, 0), (1, 1), (0, 0)))
    zc = conv_w[0] * zp[:, :-2] + conv_w[1] * zp[:, 1:-1] + conv_w[2] * zp[:, 2:]
    return gb * zc


def merge_branches(p, attn, conv_w, w_attn_o, w_conv_o, w_mix_o):
    conv = short_conv_mixer(p[..., O_CB:O_CC], p[..., O_CC:O_CX], p[..., O_CX:O_GA], conv_w)
    y = (jax.nn.sigmoid(p[..., O_GA:O_GC]) * jnp.einsum('bse,ed->bsd', attn, w_attn_o)
         + jax.nn.sigmoid(p[..., O_GC:N_IN]) * jnp.einsum('bse,ed->bsd', conv, w_conv_o))
    return jnp.einsum('bsd,de->bse', y, w_mix_o)


def hybrid_mixer(u_l, u_c, w_in, q_g, k_g, conv_w, w_attn_o, w_conv_o, w_mix_o, cos, sin, ctx_out):
    B, S = u_l.shape[0], u_l.shape[1]
    L = u_c.shape[1]
    scale = HEAD_DIM ** -0.5
    p = jnp.einsum('bsd,de->bse', u_l, w_in)
    q = rms_norm(p[..., O_Q:O_K].reshape(B, S, N_Q_HEADS, HEAD_DIM), q_g)
    k = rms_norm(p[..., O_K:O_V].reshape(B, S, N_KV_HEADS, HEAD_DIM), k_g)
    v = p[..., O_V:O_CB].reshape(B, S, N_KV_HEADS, HEAD_DIM)
    q = apply_rope(q, cos, sin) * scale
    k = apply_rope(k, cos, sin)
    if ctx_out:
        pc = jnp.einsum('bld,de->ble', u_c, w_in)
        kvc = pc[..., O_K:O_CB]
    else:
        kvc = jnp.einsum('bld,de->ble', u_c, w_in[:, O_K:O_CB])
    kc = rms_norm(kvc[..., :D_KV].reshape(B, L, N_KV_HEADS, HEAD_DIM), k_g)
    vc = kvc[..., D_KV:].reshape(B, L, N_KV_HEADS, HEAD_DIM)
    k_all = jnp.concatenate([kc, k], axis=1)
    v_all = jnp.concatenate([vc, v], axis=1)
    attn_l = latent_attention(q, k_all, v_all)
    out_l = merge_branches(p, attn_l, conv_w, w_attn_o, w_conv_o, w_mix_o)
    if not ctx_out:
        return out_l, None
    qc = rms_norm(pc[..., O_Q:O_K].reshape(B, L, N_Q_HEADS, HEAD_DIM), q_g) * scale
    attn_c = attend(qc.reshape(B, L, N_KV_HEADS, GQA_GROUP, HEAD_DIM), kc, vc).reshape(B, L, D_Q)
    out_c = merge_branches(pc, attn_c, conv_w, w_attn_o, w_conv_o, w_mix_o)
    return out_l, out_c


def moe_ffn(v, router_w, router_b, w_up, b_up, w_down, b_down):
    T, D = v.shape
    logits = jnp.einsum('td,de->te', v, router_w, preferred_element_type=jnp.float32) + router_b.astype(jnp.float32)
    top_logit, top_idx = lax.top_k(logits, TOP_K)
    gate = jax.nn.softmax(top_logit, axis=-1)
    n_assign = T * TOP_K
    flat_e = top_idx.reshape(-1)
    flat_g = gate.reshape(-1)
    flat_t = jnp.arange(n_assign, dtype=jnp.int32) // TOP_K
    order = jnp.argsort(flat_e)
    se = flat_e[order]
    counts = jnp.bincount(flat_e, length=N_EXPERTS)
    padded = (counts + EXPERT_BLOCK - 1) // EXPERT_BLOCK * EXPERT_BLOCK
    pend = jnp.cumsum(padded)
    pstart = pend - padded
    ustart = jnp.cumsum(counts) - counts
    dest = pstart[se] + jnp.arange(n_assign, dtype=jnp.int32) - ustart[se]
    n_blocks = -(-n_assign // EXPERT_BLOCK) + N_EXPERTS
    n_slots = n_blocks * EXPERT_BLOCK
    slot_tok = jnp.zeros((n_slots,), jnp.int32).at[dest].set(flat_t[order])
    slot_gate = jnp.zeros((n_slots,), jnp.float32).at[dest].set(flat_g[order])
    block_e = jnp.minimum(
        jnp.searchsorted(pend, jnp.arange(n_blocks, dtype=jnp.int32) * EXPERT_BLOCK, side='right'),
        N_EXPERTS - 1)

    def expert_block(args):
        e, tok, g = args
        xb = v[tok]
        hb = xb @ w_up[e] + b_up[e]
        glu = jnp.minimum(hb[:, :D_EXPERT], SWIGLU_LIMIT)
        lin = jnp.clip(hb[:, D_EXPERT:], -SWIGLU_LIMIT, SWIGLU_LIMIT)
        act = glu * jax.nn.sigmoid(SWIGLU_ALPHA * glu) * (lin + 1.0)
        return (act @ w_down[e] + b_down[e]) * g[:, None].astype(v.dtype)

    out = lax.map(expert_block, (block_e,
                                 slot_tok.reshape(n_blocks, EXPERT_BLOCK),
                                 slot_gate.reshape(n_blocks, EXPERT_BLOCK)))
    return jax.ops.segment_sum(out.reshape(n_slots, D), slot_tok, num_segments=T)


def setup_inputs(seed: int = 0) -> dict:
    key = jax.random.key(seed)
    ks = jax.random.split(key, 24)
    f32 = jnp.float32

    def nrm(k, shape, s):
        return jax.random.normal(k, shape, f32) * s

    D = D_MODEL
    return {
        "x": nrm(ks[0], (BATCH, SEQ, D), 1.0),
        "c": nrm(ks[1], (BATCH, D), 1.0),
        "ctx": nrm(ks[2], (BATCH, CTX_LEN, D), 1.0),
        "c_ctx": nrm(ks[3], (D,), 1.0),
        "ada_w": nrm(ks[4], (DEPTH, D, 6 * D), D ** -0.5),
        "ada_b": nrm(ks[5], (DEPTH, 6 * D), 0.02),
        "w_in": nrm(ks[6], (DEPTH, D, N_IN), D ** -0.5),
        "q_norm_g": 1.0 + nrm(ks[7], (DEPTH, HEAD_DIM), 0.02),
        "k_norm_g": 1.0 + nrm(ks[8], (DEPTH, HEAD_DIM), 0.02),
        "conv_w": nrm(ks[9], (DEPTH, CONV_WIDTH, D_CONV), CONV_WIDTH ** -0.5),
        "w_attn_o": nrm(ks[10], (DEPTH, D_Q, D), D_Q ** -0.5),
        "w_conv_o": nrm(ks[11], (DEPTH, D_CONV, D), D_CONV ** -0.5),
        "w_mix_o": nrm(ks[12], (DEPTH, D, D), DEEPNORM_BETA * D ** -0.5),
        "ln1_g": 1.0 + nrm(ks[13], (DEPTH, D), 0.02),
        "ln1_b": nrm(ks[14], (DEPTH, D), 0.02),
        "router_w": nrm(ks[15], (DEPTH, D, N_EXPERTS), D ** -0.5),
        "router_b": nrm(ks[16], (DEPTH, N_EXPERTS), 0.01),
        "w_up": nrm(ks[17], (DEPTH, N_EXPERTS, D, 2 * D_EXPERT), D ** -0.5),
        "b_up": nrm(ks[18], (DEPTH, N_EXPERTS, 2 * D_EXPERT), 0.02),
        "w_down": nrm(ks[19], (DEPTH, N_EXPERTS, D_EXPERT, D), DEEPNORM_BETA * D_EXPERT ** -0.5),
        "b_down": nrm(ks[20], (DEPTH, N_EXPERTS, D), 0.02),
        "ln2_g": 1.0 + nrm(ks[21], (DEPTH, D), 0.02),
        "ln2_b": nrm(ks[22], (DEPTH, D), 0.02),
    }


def reference(x, c, ctx, c_ctx, ada_w, ada_b, w_in, q_norm_g, k_norm_g, conv_w, w_attn_o, w_conv_o,
              w_mix_o, ln1_g, ln1_b, router_w, router_b, w_up, b_up, w_down, b_down, ln2_g, ln2_b):
    B, S, D = x.shape
    L = ctx.shape[1]
    cos, sin = axial_rope_tables(S)
    h = ctx
    silu_c = jax.nn.silu(c)
    silu_cc = jax.nn.silu(c_ctx)
    for i in range(DEPTH):
        last = i == DEPTH - 1
        mod = jnp.einsum('bd,de->be', silu_c, ada_w[i]) + ada_b[i]
        sh1, sc1, g1, sh2, sc2, g2 = [m[:, None, :] for m in jnp.split(mod, 6, axis=-1)]
        modc = silu_cc @ ada_w[i] + ada_b[i]
        sh1c, sc1c, g1c, sh2c, sc2c, g2c = jnp.split(modc, 6)

        u_l = x * (1.0 + sc1) + sh1
        u_c = h * (1.0 + sc1c) + sh1c
        mix_l, mix_c = hybrid_mixer(u_l, u_c, w_in[i], q_norm_g[i], k_norm_g[i], conv_w[i],
                                    w_attn_o[i], w_conv_o[i], w_mix_o[i], cos, sin, not last)
        x = layer_norm(DEEPNORM_ALPHA * x + g1 * mix_l, ln1_g[i], ln1_b[i])

        v_l = x * (1.0 + sc2) + sh2
        if last:
            f_l = moe_ffn(v_l.reshape(B * S, D), router_w[i], router_b[i], w_up[i], b_up[i],
                          w_down[i], b_down[i]).reshape(B, S, D)
        else:
            h = layer_norm(DEEPNORM_ALPHA * h + g1c * mix_c, ln1_g[i], ln1_b[i])
            v_c = h * (1.0 + sc2c) + sh2c
            tokens = jnp.concatenate([v_l.reshape(B * S, D), v_c.reshape(B * L, D)], axis=0)
            f = moe_ffn(tokens, router_w[i], router_b[i], w_up[i], b_up[i], w_down[i], b_down[i])
            f_l = f[:B * S].reshape(B, S, D)
            f_c = f[B * S:].reshape(B, L, D)
            h = layer_norm(DEEPNORM_ALPHA * h + g2c * f_c, ln2_g[i], ln2_b[i])
        x = layer_norm(DEEPNORM_ALPHA * x + g2 * f_l, ln2_g[i], ln2_b[i])
    return x
```

```python
import numpy as np
import concourse.bass as bass
import concourse.mybir as mybir

F32 = mybir.dt.float32
BF16 = mybir.dt.bfloat16
ALU = mybir.AluOpType
AF = mybir.ActivationFunctionType
AX = mybir.AxisListType

ENGS = ["pe", "act", "dve", "pool", "sp"]
BLOCKNAME = {"pe": "tensor", "act": "scalar", "dve": "vector", "pool": "gpsimd", "sp": "sync"}
SAME_ENG_SYNC = True


class Unit:
    __slots__ = ("name", "last_w", "reads", "sem", "ndma", "base")

    def __init__(self, name):
        self.name = name
        self.last_w = None
        self.reads = {}
        self.sem = None
        self.ndma = 0
        self.base = 0


class Op:
    __slots__ = ("eng", "fn", "waits", "is_dma", "unit", "signal", "count", "inc16", "sem")

    def __init__(self, eng, fn, is_dma=False, unit=None):
        self.eng = eng
        self.fn = fn
        self.waits = []
        self.is_dma = is_dma
        self.unit = unit
        self.signal = False
        self.count = 0
        self.inc16 = False


class Prog:
    def __init__(self, nc, stack):
        self.nc = nc
        self.stack = stack
        self.ops = []
        self.units = []
        self.last_c = {e: None for e in ENGS}
        self.esem = {}
        for e in ["pe", "act", "dve", "pool"]:
            self.esem[e] = stack.enter_context(nc.semaphore("es_" + e))
        self.nsem = 4
        self.free_sems = []

    def recycle(self, units):
        for u in units:
            if u.sem is not None:
                self.free_sems.append((u.sem, u.base + 16 * u.ndma))
                u.sem = None
        dead = set(id(u) for u in units)
        self.units = [u for u in self.units if id(u) not in dead]

    def unit(self, name):
        u = Unit(name)
        self.units.append(u)
        return u

    def _sem_for(self, u):
        if u.sem is None:
            if self.free_sems:
                u.sem, u.base = self.free_sems.pop(0)
            else:
                u.sem = self.stack.enter_context(self.nc.semaphore("us_%d" % self.nsem))
                self.nsem += 1
                u.base = 0
        return u.sem

    def _dep(self, op, prod, war=False):
        if prod.is_dma:
            return
        if prod.eng == op.eng:
            if op.eng == "pe" or war or not SAME_ENG_SYNC:
                return
        prod.signal = True
        op.waits.append(("op", prod))

    def _dma_wait(self, op, u):
        if u.ndma > 0:
            op.waits.append(("dma", u.sem, u.base + 16 * u.ndma))

    def add(self, eng, fn, reads=(), writes=()):
        op = Op(eng, fn)
        for u in reads:
            lw = u.last_w
            if lw is not None:
                if lw.is_dma:
                    self._dma_wait(op, u)
                else:
                    self._dep(op, lw)
        for u in writes:
            lw = u.last_w
            if lw is not None and not lw.is_dma:
                self._dep(op, lw)
            self._dma_wait(op, u)
            for e, r in u.reads.items():
                self._dep(op, r, war=True)
        for u in reads:
            u.reads[eng] = op
        for u in writes:
            u.last_w = op
            u.reads = {}
        self.ops.append(op)
        self.last_c[eng] = op
        return op

    def dma(self, q, out, in_, unit, load, extra_reads=(), **kw):
        op = Op(q, lambda e: e.dma_start(out=out, in_=in_, **kw), is_dma=True, unit=unit)
        u = unit
        op.sem = self._sem_for(u)
        if load:
            lw = u.last_w
            if lw is not None and not lw.is_dma:
                self._dep(op, lw)
            self._dma_wait(op, u)
            for e, r in u.reads.items():
                self._dep(op, r)
            u.last_w = op
            u.reads = {}
        else:
            lw = u.last_w
            if lw is not None:
                if lw.is_dma:
                    self._dma_wait(op, u)
                else:
                    self._dep(op, lw)
        for u2 in extra_reads:
            lw = u2.last_w
            if lw is not None:
                if lw.is_dma:
                    self._dma_wait(op, u2)
                else:
                    self._dep(op, lw)
        u.ndma += 1
        self.ops.append(op)
        return op

    def raw16(self, q, fn, unit):
        op = Op(q, fn, is_dma=True, unit=unit)
        op.sem = self._sem_for(unit)
        self._dma_wait(op, unit)
        unit.last_w = op
        unit.ndma += 1
        self.ops.append(op)
        return op

    def wait_unit(self, eng, u):
        op = Op(eng, None)
        self._dma_wait(op, u)
        self.ops.append(op)

    def barrier(self):
        snap = dict(self.last_c)
        for e, p in snap.items():
            if p is not None:
                p.signal = True
        dmas = [(u.sem, u.base + 16 * u.ndma) for u in self.units if u.ndma > 0 and u.sem is not None]
        for e in ENGS:
            op = Op(e, None)
            for e2, p in snap.items():
                if p is not None and e2 != e:
                    op.waits.append(("op", p))
            for u, v in dmas:
                op.waits.append(("dma", u, v))
            self.ops.append(op)

    def emit(self):
        cnt = {e: 0 for e in ENGS}
        for op in self.ops:
            if op.signal and not op.is_dma:
                cnt[op.eng] += 1
                op.count = cnt[op.eng]
        nc = self.nc
        stats = {e: [0, 0] for e in ENGS}
        with nc.Block() as block:
            for e in ENGS:
                def body(engobj, e=e):
                    seen = {}
                    for op in self.ops:
                        if op.eng != e:
                            continue
                        for w in op.waits:
                            if w[0] == "op":
                                sem, val, key = self.esem[w[1].eng], w[1].count, w[1].eng
                            else:
                                sem, val, key = w[1], w[2], id(w[1])
                            if seen.get(key, 0) >= val:
                                continue
                            engobj.wait_ge(sem, val)
                            seen[key] = val
                            stats[e][1] += 1
                        if op.fn is not None:
                            inst = op.fn(engobj)
                            stats[e][0] += 1
                            if op.is_dma:
                                inst.then_inc(op.sem, 16)
                            elif op.signal:
                                inst.then_inc(self.esem[e], 1)
                getattr(block, BLOCKNAME[e])(body)
        return stats, cnt


class SB:
    def __init__(self, prog, nwords):
        self.p = prog
        self.nw = nwords
        self.big = prog.stack.enter_context(prog.nc.sbuf_tensor("big", [128, nwords], F32))
        self.off = 0
        self.peak = 0
        self.allocs = []

    def mark(self):
        return self.off

    def release(self, m):
        dead = []
        while self.allocs and self.allocs[-1][0] >= m:
            dead.extend(self.allocs.pop()[1].us)
        self.p.recycle(dead)
        self.off = m

    def alloc(self, name, free_shape, dtype=F32, nunits=1):
        n = int(np.prod(free_shape))
        esz = 4 if dtype == F32 else 2
        words = (n * esz + 3) // 4
        words = (words + 7) // 8 * 8
        assert self.off + words <= self.nw, "SBUF overflow %s %d+%d>%d" % (name, self.off, words, self.nw)
        ap = self.big[:, self.off:self.off + words]
        if dtype != F32:
            ap = ap.bitcast(dtype)
        ap = ap[:, 0:n]
        if len(free_shape) == 2:
            ap = ap.rearrange("p (a b) -> p a b", b=free_shape[1])
        elif len(free_shape) == 3:
            ap = ap.rearrange("p (a b c) -> p a b c", b=free_shape[1], c=free_shape[2])
        self.off += words
        self.peak = max(self.peak, self.off)
        t = T(ap, [self.p.unit("%s.%d" % (name, i)) for i in range(nunits)])
        self.allocs.append((self.off - words, t))
        return t


class T:
    __slots__ = ("ap", "us")

    def __init__(self, ap, us):
        self.ap = ap
        self.us = us

    @property
    def u(self):
        return self.us[0]


class PS:
    def __init__(self, prog):
        self.p = prog
        self.banks = []
        for i in range(8):
            t = prog.stack.enter_context(prog.nc.psum_tensor("psb%d" % i, [128, 512], F32))
            self.banks.append(T(t[:, :], [prog.unit("ps%d" % i)]))
        self.i = 0

        self.rot = list(self.banks)

    def get(self):
        b = self.rot[self.i % len(self.rot)]
        self.i += 1
        return b

    def hold(self):
        b = self.rot.pop(self.i % len(self.rot))
        return b

    def unhold(self, b):
        self.rot.append(b)

from contextlib import ExitStack
from concourse.bass_utils import run_bass_kernel_spmd

NORM_EPS = 1e-6
SW_ALPHA = 1.702
SW_LIMIT = 7.0


class Cfg:
    def __init__(self, D, S, L, E, DEPTH=2):
        self.D, self.S, self.L, self.E, self.DEPTH = D, S, L, E, DEPTH
        self.KC = D // 128
        self.H = self.KC
        self.HKV = self.H // 4
        self.DKV = self.HKV * 128
        self.F = D // 2
        self.FC = self.F // 128
        self.NIN = 6 * D + 2 * self.DKV
        self.T = S + L
        self.NT = self.T // 128
        self.NTL = S // 128
        self.CW = min(512, D)
        self.O_K = D
        self.O_V = D + self.DKV
        self.O_CB = D + 2 * self.DKV
        self.O_CC = self.O_CB + D
        self.O_CX = self.O_CC + D
        self.O_GA = self.O_CX + D
        self.O_GC = self.O_GA + D
        self.blocks = []
        for t0 in range(0, S, 512):
            self.blocks.append((t0, min(512, S - t0), False))
        for t0 in range(S, self.T, 512):
            self.blocks.append((t0, min(512, self.T - t0), True))
        self.alpha = (2 * DEPTH) ** 0.25


def build(cfg, nphase=99, debug=()):
    import os
    MOE_CUT = int(os.environ.get('MOE_CUT', '9'))
    D, S, L, E, DEPTH = cfg.D, cfg.S, cfg.L, cfg.E, cfg.DEPTH
    KC, H, HKV, DKV, F, FC, NIN, T, NT, NTL, CW = (cfg.KC, cfg.H, cfg.HKV, cfg.DKV, cfg.F, cfg.FC,
                                                   cfg.NIN, cfg.T, cfg.NT, cfg.NTL, cfg.CW)
    NJ = CW // 128
    blocks = cfg.blocks
    nc = bass.Bass("TRN2", target_bir_lowering=False)

    def din(name, shape, dt=F32):
        return nc.dram_tensor(name, list(shape), dt, kind="ExternalInput").ap()

    def dint(name, shape, dt):
        return nc.dram_tensor(name, list(shape), dt).ap()

    x_in = din("x", [S, D])
    ctx_in = din("ctx", [L, D])
    c_in = din("c", [1, D])
    cctx_in = din("c_ctx", [1, D])
    ada_w = din("ada_w", [DEPTH, D, 6 * D])
    ada_b = din("ada_b", [DEPTH, 6 * D])
    w_in = din("w_in", [DEPTH, D, NIN])
    qg_in = din("q_norm_g", [DEPTH, 128])
    kg_in = din("k_norm_g", [DEPTH, 128])
    convw_in = din("conv_w", [DEPTH, 3, D])
    w_ao = din("w_attn_o", [DEPTH, D, D])
    w_co = din("w_conv_o", [DEPTH, D, D])
    w_mo = din("w_mix_o", [DEPTH, D, D])
    ln1g = din("ln1_g", [DEPTH, D])
    ln1b = din("ln1_b", [DEPTH, D])
    rw_in = din("router_w", [DEPTH, D, E])
    rb_in = din("router_b", [DEPTH, E])
    w_up = din("w_up", [DEPTH, E, D, 2 * F])
    b_up = din("b_up", [DEPTH, E, 2 * F])
    w_dn = din("w_down", [DEPTH, E, F, D])
    b_dn = din("b_down", [DEPTH, E, D])
    ln2g = din("ln2_g", [DEPTH, D])
    ln2b = din("ln2_b", [DEPTH, D])
    ident_in = din("ident", [128, 128])
    cos_in = din("cos", [S, 64])
    sin_in = din("sin", [S, 64])
    out_d = nc.dram_tensor("out", [S, D], F32, kind="ExternalOutput").ap()

    ada_wb = dint("ada_wb", [DEPTH, D, 6 * D], BF16)
    w_inb = dint("w_inb", [DEPTH, D, NIN], BF16)
    w_aob = dint("w_aob", [DEPTH, D, D], BF16)
    w_cob = dint("w_cob", [DEPTH, D, D], BF16)
    w_mob = dint("w_mob", [DEPTH, D, D], BF16)
    EH = E // 2
    w_upb = [[dint("w_upb_%d_%d" % (l_, h_), [EH, D, 2 * F], BF16) for h_ in range(2)] for l_ in range(DEPTH)]
    w_dnb = [dint("w_dnb_%d" % l_, [E, F, D], BF16) for l_ in range(DEPTH)]
    qT_d = dint("qT_d", [D, T], BF16)
    kT_d = dint("kT_d", [DKV, T], BF16)
    V_d = dint("V_d", [T, DKV], BF16)
    convT_d = dint("convT_d", [D, T], BF16)
    gaT_d = dint("gaT_d", [D, T], BF16)
    gcT_d = dint("gcT_d", [D, T], BF16)
    attnT_d = dint("attnT_d", [D, T], BF16)
    x1_d = dint("x1_d", [T, D], F32)
    xs_d = dint("xs_d", [T, D], F32)
    grow_d = dint("grow_d", [DEPTH, 4, D], F32)

    stack = ExitStack()
    P = Prog(nc, stack)
    sb = SB(P, 51800)
    ps = PS(P)

    def TT(eng, out, in0, in1, op, R, W):
        P.add(eng, lambda e: e.tensor_tensor(out=out, in0=in0, in1=in1, op=op), R, W)

    def TS(eng, out, in0, s1, s2, op0, op1, R, W):
        if op1 is None:
            P.add(eng, lambda e: e.tensor_scalar(out=out, in0=in0, scalar1=s1, scalar2=None, op0=op0), R, W)
        else:
            P.add(eng, lambda e: e.tensor_scalar(out=out, in0=in0, scalar1=s1, scalar2=s2, op0=op0, op1=op1), R, W)

    def STT(eng, out, in0, scalar, in1, op0, op1, R, W):
        P.add(eng, lambda e: e.scalar_tensor_tensor(out=out, in0=in0, scalar=scalar, in1=in1, op0=op0, op1=op1), R, W)

    def ACT(out, in_, func, R, W, bias=0.0, scale=1.0, accum=None):
        if accum is None:
            P.add("act", lambda e: e.activation(out=out, in_=in_, func=func, bias=bias, scale=scale), R, W)
        else:
            P.add("act", lambda e: e.activation(out=out, in_=in_, func=func, bias=bias, scale=scale, accum_out=accum), R, W)

    def CP(eng, out, in_, R, W):
        if eng == "act":
            P.add("act", lambda e: e.copy(out=out, in_=in_), R, W)
        else:
            P.add(eng, lambda e: e.tensor_copy(out=out, in_=in_), R, W)

    def MM(out, lhsT, rhs, start, stop, R, W):
        P.add("pe", lambda e: e.matmul(out, lhsT=lhsT, rhs=rhs, start=start, stop=stop), R, W)

    def TR(out, in_, ident, R, W):
        P.add("pe", lambda e: e.transpose(out=out, in_=in_, identity=ident), R, W)

    def LD(dst_t, dst_ap, src, extra=(), q="sp"):
        P.dma(q, dst_ap, src, dst_t, True, extra_reads=extra)

    def ST(dst, src_ap, src_u, q="act"):
        P.dma(q, dst, src_ap, src_u, False)

    class Pool:
        def __init__(self, name, n, shape, dt):
            self.ts = [sb.alloc("%s%d" % (name, i), shape, dt) for i in range(n)]
            self.i = 0

        def get(self):
            t = self.ts[self.i % len(self.ts)]
            self.i += 1
            return t

    rr = {"n": 0}

    def alt(engs):
        rr["n"] += 1
        return engs[rr["n"] % len(engs)]

    cast_u = {}

    def cast(name, l, src2d, dst2d, rows, rchunk):
        u = P.unit("cast_%s%d" % (name, l))
        cast_u[(name, l)] = u
        for r0 in range(0, rows, rchunk):
            r1 = min(rows, r0 + rchunk)
            P.raw16("pool", lambda e, r0=r0, r1=r1: e.dma_start(out=dst2d[r0:r1, :], in_=src2d[r0:r1, :]), u)

    NG = 8
    EG = E // NG

    def cast_rows(u, src2d, dst2d, rows, rchunk):
        for r0 in range(0, rows, rchunk):
            r1 = min(rows, r0 + rchunk)
            P.raw16("pool", lambda e, r0=r0, r1=r1: e.dma_start(out=dst2d[r0:r1, :], in_=src2d[r0:r1, :]), u)

    for l in range(DEPTH):
        if l > 0:
            cast("ada", l, ada_w[l], ada_wb[l], D, 128)
            cast("win", l, w_in[l], w_inb[l], D, 128)
        cast("wao", l, w_ao[l], w_aob[l], D, 512)
        cast("wco", l, w_co[l], w_cob[l], D, 512)
        cast("wmo", l, w_mo[l], w_mob[l], D, 512)
        for g_ in range(NG):
            e0 = g_ * EG
            hh, i0 = e0 // EH, e0 % EH
            u = P.unit("cast_wup%d_%d" % (l, g_))
            cast_u[("wup", l, g_)] = u
            cast_rows(u, w_up[l, e0:e0 + EG].rearrange("e d f -> (e d) f"),
                      w_upb[l][hh][i0:i0 + EG].rearrange("e d f -> (e d) f"), EG * D, 512)
            u = P.unit("cast_wdn%d_%d" % (l, g_))
            cast_u[("wdn", l, g_)] = u
            cast_rows(u, w_dn[l, e0:e0 + EG].rearrange("e f d -> (e f) d"),
                      w_dnb[l][e0:e0 + EG].rearrange("e f d -> (e f) d"), EG * F, 512)

    ident = sb.alloc("ident", (128,), F32)
    LD(ident.u, ident.ap, ident_in)
    identb = sb.alloc("identb", (128,), BF16)
    CP("dve", identb.ap, ident.ap, [ident.u], [identb.u])
    onesf = sb.alloc("onesf", (128,), F32)
    P.add("dve", lambda e: e.memset(onesf.ap, 1.0), [], [onesf.u])
    onesb = sb.alloc("onesb", (128,), BF16)
    P.add("dve", lambda e: e.memset(onesb.ap, 1.0), [], [onesb.u])

    def load_T(dst_ap, rows_ap, R, dst_u, eng="dve"):
        m = sb.mark()
        tmp = sb.alloc("ldT", (128,), F32)
        LD(tmp.u, tmp.ap[:R, :], rows_ap)
        b = ps.get()
        TR(b.ap[:, 0:R], tmp.ap[:R, :], ident.ap[:R, :R], [tmp.u, ident.u], [b.u])
        CP(eng, dst_ap, b.ap[:, 0:R], [b.u], [dst_u])
        return m

    silT = sb.alloc("silT", (2, KC), F32)
    for v, src in ((0, c_in), (1, cctx_in)):
        tmp = sb.alloc("ctmp%d" % v, (128,), F32)
        LD(tmp.u, tmp.ap[:KC, :], src.rearrange("o (kc p) -> (o kc) p", p=128))
        b = ps.get()
        TR(b.ap[:, 0:KC], tmp.ap[:KC, :], ident.ap[:KC, :KC], [tmp.u, ident.u], [b.u])
        ACT(silT.ap[:, v, :], b.ap[:, 0:KC], AF.Silu, [b.u], [silT.u])
    sil = sb.alloc("sil", (KC, 2), BF16)
    for v in range(2):
        CP("dve", sil.ap[:, :, v], silT.ap[:, v, :], [silT.u], [sil.u])
    base_mark = sb.mark()

    def xsrc(l, t0, n):
        if l == 0:
            if t0 < S:
                return x_in[t0:t0 + n, :]
            return ctx_in[t0 - S:t0 - S + n, :]
        return xs_d[t0:t0 + n, :]

    stage_ref = {}

    def wload(pool, w2d, c0, cw, cu, direct=None):
        t = pool.get()
        if direct is None:
            LD(t.u, t.ap[:, :, :cw], w2d[:, c0:c0 + cw].rearrange("(kc p) c -> p kc c", p=128), extra=[cu])
        else:
            KH = KC // 4
            for hf in range(4):
                st = stage_ref["p"].get()
                k0 = hf * KH
                LD(st.u, st.ap[:, :, :cw],
                   direct[k0 * 128:(k0 + KH) * 128, c0:c0 + cw].rearrange("(kc p) c -> p kc c", p=128))
                CP(alt(["act", "pool"]), t.ap[:, k0:k0 + KH, :cw], st.ap[:, :, :cw], [st.u], [t.u])
        return t

    def mm_fm(pb, n, wt, c0, rhs_ap_fn, units):
        for kc in range(KC):
            MM(pb.ap[:, :n], wt.ap[:, kc, c0:c0 + 128], rhs_ap_fn(kc), kc == 0, kc == KC - 1, [wt.u] + units, [pb.u])

    phase = {"n": 0}

    def phase_end():
        P.barrier()
        phase["n"] += 1
        return phase["n"] >= nphase

    scratch = {"qT_d": qT_d, "kT_d": kT_d, "V_d": V_d, "convT_d": convT_d, "gaT_d": gaT_d, "gcT_d": gcT_d,
               "attnT_d": attnT_d, "x1_d": x1_d, "xs_d": xs_d, "grow_d": grow_d}

    def finish():
        P.barrier()
        for name in debug:
            src = scratch[name]
            dst = nc.dram_tensor("dbg_" + name, list(src.shape), F32, kind="ExternalOutput").ap()
            du = P.unit("dbg_" + name)
            if len(src.shape) == 3:
                for i in range(src.shape[0]):
                    P.raw16("pool", lambda e, i=i, dst=dst, src=src: e.dma_start(out=dst[i], in_=src[i]), du)
            else:
                P.raw16("pool", lambda e, dst=dst, src=src: e.dma_start(out=dst, in_=src), du)
        P.barrier()
        st = P.emit()
        print("emit stats", st, "sbuf peak words", sb.peak, "nsem", P.nsem, flush=True)
        stack.close()
        return nc

    for l in range(DEPTH):
        last = (l == DEPTH - 1)
        lblocks = [b for b in blocks if not (last and b[2])]
        sb.release(base_mark)
        modA = sb.alloc("modA", (6 * KC, 2), F32)
        p1mark = sb.mark()
        silrep = sb.alloc("silrep", (2, KC, 128), BF16)
        for v in range(2):
            for kc in range(KC):
                TS("dve", silrep.ap[:, v, kc, :], onesf.ap, silT.ap[:, v, kc:kc + 1], None, ALU.mult, None,
                   [onesf.u, silT.u], [silrep.u])
        adabT = sb.alloc("adabT", (6 * KC,), F32)
        load_T(adabT.ap, ada_b[l].rearrange("(j p) -> j p", p=128), 6 * KC, adabT.u)
        wp = Pool("w1_", 3, (KC, CW), BF16)
        if l == 0:
            stage_ref["p"] = Pool("stg1_", 2, (KC // 4, CW), F32)
        growp = Pool("grow", 2, (CW,), F32)
        abp = Pool("abrow", 2, (CW,), F32)
        modps = ps.hold()
        for b in range(6 * D // CW):
            wt = wload(wp, ada_wb[l], b * CW, CW, cast_u.get(("ada", l)), direct=(ada_w[l] if l == 0 else None))
            for jl in range(NJ):
                j = b * NJ + jl
                for kc in range(KC):
                    MM(modps.ap[:, 2 * j:2 * j + 2], wt.ap[:, kc, jl * 128:(jl + 1) * 128], sil.ap[:, kc, :],
                       kc == 0, kc == KC - 1, [wt.u, sil.u], [modps.u])
            sec = (b * CW) // D
            if sec in (2, 5):
                for v in range(2):
                    if last and v == 1:
                        continue
                    pb = ps.get()
                    for kc in range(KC):
                        MM(pb.ap[:, 0:CW], silrep.ap[:, v, kc, :], wt.ap[:, kc, :], kc == 0, kc == KC - 1,
                           [wt.u, silrep.u], [pb.u])
                    ab = abp.get()
                    LD(ab.u, ab.ap[0:1, :], ada_b[l:l + 1, b * CW:(b + 1) * CW])
                    gr = growp.get()
                    TT("dve", gr.ap[0:1, :], pb.ap[0:1, 0:CW], ab.ap[0:1, :], ALU.add, [pb.u, ab.u], [gr.u])
                    c0 = b * CW - sec * D
                    ST(grow_d[l, (2 if sec == 5 else 0) + v:(2 if sec == 5 else 0) + v + 1, c0:c0 + CW], gr.ap[0:1, :], gr.u)
        mview = modps.ap[:, 0:12 * KC].rearrange("p (j v) -> p j v", v=2)
        for v in range(2):
            TT("dve", modA.ap[:, :, v], mview[:, :, v], adabT.ap, ALU.add, [modps.u, adabT.u], [modA.u])
        for sec in (1, 4):
            TS("dve", modA.ap[:, sec * KC:(sec + 1) * KC, :], modA.ap[:, sec * KC:(sec + 1) * KC, :], 1.0, None,
               ALU.add, None, [modA.u], [modA.u])
        ps.unhold(modps)
        if phase_end():
            return finish()
        sb.release(p1mark)

        def mod_ap(sec, kc, v):
            return modA.ap[:, sec * KC + kc, v:v + 1]

        def transpose_modulate(dstT, src_rows_fn, tiles, sec_sh, sec_sc, xpool, f32dst=None):
            for (i, col, v, du) in tiles:
                xt = xpool.get()
                LD(xt.u, xt.ap, src_rows_fn(i))
                for kg in range(0, KC, 4):
                    b = ps.get()
                    nk = min(4, KC - kg)
                    for q in range(nk):
                        TR(b.ap[:, q * 128:(q + 1) * 128], xt.ap[:, (kg + q) * 128:(kg + q + 1) * 128], ident.ap,
                           [xt.u, ident.u], [b.u])
                    for q in range(nk):
                        kc = kg + q
                        eng = alt(["act", "dve"])
                        o = dstT.ap[:, kc, col:col + 128]
                        src = b.ap[:, q * 128:(q + 1) * 128]
                        if f32dst is not None:
                            tgt, tu = f32dst[0].ap[:, kc, :], f32dst[0].u
                        else:
                            tgt, tu = o, du
                        if eng == "act":
                            ACT(tgt, src, AF.Identity, [b.u, modA.u], [tu],
                                bias=mod_ap(sec_sh, kc, v), scale=mod_ap(sec_sc, kc, v))
                        else:
                            TS("dve", tgt, src, mod_ap(sec_sc, kc, v), mod_ap(sec_sh, kc, v),
                               ALU.mult, ALU.add, [b.u, modA.u], [tu])
                        if f32dst is not None:
                            CP("pool", o, tgt, [tu], [du])
                if f32dst is not None:
                    f32dst[1](i, col, v)

        p2mark = sb.mark()
        uT = sb.alloc("uT", (KC, T), BF16, nunits=len(blocks))

        def blk_of(tok):
            for bi, (t0, n, isc) in enumerate(blocks):
                if t0 <= tok < t0 + n:
                    return bi
        xpool = Pool("xt", 2, (D,), F32)
        tiles = [(i, i * 128, 1 if i >= NTL else 0, uT.us[blk_of(i * 128)]) for i in range(NT)]
        transpose_modulate(uT, lambda i: xsrc(l, i * 128, 128), tiles, 0, 1, xpool)

        wp = Pool("w3_", 3, (KC, CW), BF16)
        wcu = cast_u.get(("win", l))
        if l == 0:
            stage_ref["p"] = Pool("stg3_", 2, (KC // 4, CW), F32)

        def wl3(c0, cw):
            return wload(wp, w_inb[l], c0, cw, wcu, direct=(w_in[l] if l == 0 else None))
        m3 = sb.mark()
        cosT = sb.alloc("cosT", (NTL, 64), F32)
        sinT = sb.alloc("sinT", (NTL, 64), F32)
        LD(cosT.u, cosT.ap, cos_in.rearrange("(i p) f -> p i f", p=128))
        LD(sinT.u, sinT.ap, sin_in.rearrange("(i p) f -> p i f", p=128))
        grow_q = sb.alloc("grow_q", (128,), F32)
        grow_k = sb.alloc("grow_k", (128,), F32)
        LD(grow_q.u, grow_q.ap, qg_in[l:l + 1, :].partition_broadcast(128))
        LD(grow_k.u, grow_k.ap, kg_in[l:l + 1, :].partition_broadcast(128))
        TS("dve", grow_q.ap, grow_q.ap, float(128 ** -0.5), None, ALU.mult, None, [grow_q.u], [grow_q.u])
        sqp = Pool("sq", 2, (CW,), F32)
        xnp = Pool("xn", 2, (CW,), F32)
        ssp = Pool("ss", 2, (8,), F32)
        rp_a = Pool("rpa", 2, (CW // 2,), F32)
        rp_b = Pool("rpb", 2, (CW // 2,), F32)
        qnp = Pool("qn", 2, (CW,), BF16)
        stgp = Pool("qstg", 2, (NJ, 512), BF16)

        def qk_chain(pb, cw, i, grow, rope):
            nh = cw // 128
            sq = sqp.get()
            ACT(sq.ap[:, :cw], pb.ap[:, :cw], AF.Square, [pb.u], [sq.u])
            ss = ssp.get()
            P.add("dve", lambda e: e.tensor_reduce(out=ss.ap[:, :nh], in_=sq.ap[:, :cw].rearrange("p (h d) -> p h d", d=128),
                                                   axis=AX.X, op=ALU.add), [sq.u], [ss.u])
            ACT(ss.ap[:, :nh], ss.ap[:, :nh], AF.Sqrt, [ss.u], [ss.u], bias=NORM_EPS, scale=1.0 / 128)
            P.add("dve", lambda e: e.reciprocal(out=ss.ap[:, :nh], in_=ss.ap[:, :nh]), [ss.u], [ss.u])
            xn = xnp.get()
            for hh in range(nh):
                STT("dve", xn.ap[:, hh * 128:(hh + 1) * 128], pb.ap[:, hh * 128:(hh + 1) * 128], ss.ap[:, hh:hh + 1],
                    grow.ap, ALU.mult, ALU.mult, [pb.u, ss.u, grow.u], [xn.u])
            qn = qnp.get()
            if not rope:
                CP("act", qn.ap[:, :cw], xn.ap[:, :cw], [xn.u], [qn.u])
                return qn
            xv = xn.ap[:, :cw].rearrange("p (h f two) -> p h f two", two=2, f=64)
            qv = qn.ap[:, :cw].rearrange("p (h f two) -> p h f two", two=2, f=64)
            ta = rp_a.get()
            tb = rp_b.get()
            for hh in range(nh):
                x0 = xv[:, hh, :, 0]
                x1 = xv[:, hh, :, 1]
                a = ta.ap[:, hh * 64:(hh + 1) * 64]
                bq = tb.ap[:, hh * 64:(hh + 1) * 64]
                cs = cosT.ap[:, i, :]
                sn = sinT.ap[:, i, :]
                TT("dve", a, x0, cs, ALU.mult, [xn.u, cosT.u], [ta.u])
                TT("pool", bq, x1, sn, ALU.mult, [xn.u, sinT.u], [tb.u])
                TT("dve", qv[:, hh, :, 0], a, bq, ALU.subtract, [ta.u, tb.u], [qn.u])
                TT("pool", a, x0, sn, ALU.mult, [xn.u, sinT.u, qn.u], [ta.u])
                TT("dve", bq, x1, cs, ALU.mult, [xn.u, cosT.u, qn.u], [tb.u])
                TT("pool", qv[:, hh, :, 1], a, bq, ALU.add, [ta.u, tb.u], [qn.u])
            return qn

        def qk_group(c0, ncols, grow, dstT_d, is_q):
            for cb0 in range(0, ncols, CW):
                cw = min(CW, ncols - cb0)
                nh = cw // 128
                wt = wl3(c0 + cb0, cw)
                for bi, (t0, n, isc) in enumerate(blocks):
                    stg = stgp.get()
                    for jt in range(n // 128):
                        i = t0 // 128 + jt
                        pb = ps.get()
                        for kc in range(KC):
                            MM(pb.ap[:, :cw], uT.ap[:, kc, i * 128:(i + 1) * 128], wt.ap[:, kc, :cw], kc == 0, kc == KC - 1,
                               [uT.us[bi], wt.u], [pb.u])
                        qn = qk_chain(pb, cw, i, grow, rope=not isc)
                        pt = ps.get()
                        ptb = pt.ap.bitcast(BF16)
                        for hh in range(nh):
                            TR(ptb[:, hh * 128:(hh + 1) * 128], qn.ap[:, hh * 128:(hh + 1) * 128], identb.ap,
                               [qn.u, identb.u], [pt.u])
                        CP(alt(["act", "dve"]), stg.ap[:, :nh, jt * 128:(jt + 1) * 128],
                           ptb[:, 0:cw].rearrange("p (h t) -> p h t", t=128), [pt.u], [stg.u])
                    ST(dstT_d[cb0:cb0 + cw, t0:t0 + n].rearrange("(h p) t -> p h t", p=128), stg.ap[:, :nh, :n], stg.u)

        qk_group(0, D, grow_q, qT_d, True)
        qk_group(cfg.O_K, DKV, grow_k, kT_d, False)
        vstp = Pool("vst", 2, (CW,), BF16)
        for cb0 in range(0, DKV, CW):
            cw = min(CW, DKV - cb0)
            wt = wl3(cfg.O_V + cb0, cw)
            for bi, (t0, n, isc) in enumerate(blocks):
                for jt in range(n // 128):
                    i = t0 // 128 + jt
                    pb = ps.get()
                    for kc in range(KC):
                        MM(pb.ap[:, :cw], uT.ap[:, kc, i * 128:(i + 1) * 128], wt.ap[:, kc, :cw], kc == 0, kc == KC - 1,
                           [uT.us[bi], wt.u], [pb.u])
                    vs = vstp.get()
                    CP(alt(["act", "dve"]), vs.ap[:, :cw], pb.ap[:, :cw], [pb.u], [vs.u])
                    ST(V_d[i * 128:(i + 1) * 128, cb0:cb0 + cw], vs.ap[:, :cw], vs.u)
        P.barrier()
        sb.release(m3)
        W = T + 3

        def zcol(t):
            return t + 1 if t < S else t + 2
        cwT = sb.alloc("cwT", (3 * KC,), F32)
        load_T(cwT.ap, convw_in[l].rearrange("j (kc p) -> (j kc) p", p=128), 3 * KC, cwT.u)
        zp = Pool("z", 2, (W,), F32)
        for zt in zp.ts:
            P.add("dve", lambda e, zt=zt: e.memset(zt.ap, 0.0), [], [zt.u])
        cvp = Pool("cv", 1, (W,), F32)
        csp = Pool("cS", 2, (512,), F32)
        cstp = Pool("cst", 2, (T,), BF16)
        for cb in range(D // CW):
            wC = wl3(cfg.O_CC + cb * CW, CW)
            wX = wl3(cfg.O_CX + cb * CW, CW)
            wB = wl3(cfg.O_CB + cb * CW, CW)
            for dcl in range(NJ):
                dc = cb * NJ + dcl
                z = zp.get()
                for bi, (t0, n, isc) in enumerate(blocks):
                    pC = ps.get()
                    mm_fm(pC, n, wC, dcl * 128, lambda kc: uT.ap[:, kc, t0:t0 + n], [uT.us[bi]])
                    pX = ps.get()
                    mm_fm(pX, n, wX, dcl * 128, lambda kc: uT.ap[:, kc, t0:t0 + n], [uT.us[bi]])
                    cS = csp.get()
                    CP("act", cS.ap[:, :n], pC.ap[:, :n], [pC.u], [cS.u])
                    TT("dve", z.ap[:, zcol(t0):zcol(t0) + n], pX.ap[:, :n], cS.ap[:, :n], ALU.mult, [pX.u, cS.u], [z.u])
                cv = cvp.get()
                ACT(cv.ap[:, 1:W - 1], z.ap[:, 1:W - 1], AF.Identity, [z.u, cwT.u], [cv.u], scale=cwT.ap[:, KC + dc:KC + dc + 1])
                STT("dve", cv.ap[:, 1:W - 1], z.ap[:, 0:W - 2], cwT.ap[:, dc:dc + 1], cv.ap[:, 1:W - 1], ALU.mult, ALU.add,
                    [z.u, cwT.u, cv.u], [cv.u])
                STT("dve", cv.ap[:, 1:W - 1], z.ap[:, 2:W], cwT.ap[:, 2 * KC + dc:2 * KC + dc + 1], cv.ap[:, 1:W - 1],
                    ALU.mult, ALU.add, [z.u, cwT.u, cv.u], [cv.u])
                cst = cstp.get()
                for bi, (t0, n, isc) in enumerate(blocks):
                    pB = ps.get()
                    mm_fm(pB, n, wB, dcl * 128, lambda kc: uT.ap[:, kc, t0:t0 + n], [uT.us[bi]])
                    TT("dve", cst.ap[:, t0:t0 + n], pB.ap[:, :n], cv.ap[:, zcol(t0):zcol(t0) + n], ALU.mult, [pB.u, cv.u], [cst.u])
                ST(convT_d[dc * 128:(dc + 1) * 128, :], cst.ap, cst.u)
        P.barrier()
        sb.release(m3)
        gstp = Pool("gst", 2, (T,), BF16)
        for (og, gd) in ((cfg.O_GA, gaT_d), (cfg.O_GC, gcT_d)):
            for cb in range(D // CW):
                wt = wl3(og + cb * CW, CW)
                for dcl in range(NJ):
                    dc = cb * NJ + dcl
                    gst = gstp.get()
                    for bi, (t0, n, isc) in enumerate(blocks):
                        pg = ps.get()
                        mm_fm(pg, n, wt, dcl * 128, lambda kc: uT.ap[:, kc, t0:t0 + n], [uT.us[bi]])
                        ACT(gst.ap[:, t0:t0 + n], pg.ap[:, :n], AF.Sigmoid, [pg.u], [gst.u])
                    ST(gd[dc * 128:(dc + 1) * 128, :], gst.ap, gst.u)
        if phase_end():
            return finish()
        sb.release(p2mark)

        kT = sb.alloc("kT", (HKV, T), BF16)
        LD(kT.u, kT.ap, kT_d.rearrange("(g p) t -> p g t", p=128))
        Vt = sb.alloc("Vt", (NT, DKV), BF16)
        LD(Vt.u, Vt.ap, V_d.rearrange("(i p) c -> p i c", p=128))
        Tq = S if last else T
        qhp = Pool("qh", 2, (T,), BF16)
        ptp = Pool("PT", 4, (512,), BF16)
        rdp = Pool("rden", 2, (512,), F32)
        aop = Pool("attn_o", 2, (T,), BF16)
        for h in range(H):
            g = h // 4
            qh = qhp.get()
            LD(qh.u, qh.ap[:, :Tq], qT_d[h * 128:(h + 1) * 128, 0:Tq])
            ao = aop.get()
            for (t0, n, isc) in blocks:
                if isc and last:
                    continue
                kts = list(range(NTL, NT)) if isc else list(range(NT))
                pO = ps.hold()
                pD = ps.hold()
                sbanks = []

                def emit_S(kt):
                    pS = ps.get()
                    MM(pS.ap[:, :n], kT.ap[:, g, kt * 128:(kt + 1) * 128], qh.ap[:, t0:t0 + n], True, True, [kT.u, qh.u], [pS.u])
                    sbanks.append(pS)
                emit_S(kts[0])
                for idx, kt in enumerate(kts):
                    if idx + 1 < len(kts):
                        emit_S(kts[idx + 1])
                    pS = sbanks[idx]
                    pt = ptp.get()
                    ACT(pt.ap[:, :n], pS.ap[:, :n], AF.Exp, [pS.u], [pt.u])
                    MM(pO.ap[:, :n], Vt.ap[:, kt, g * 128:(g + 1) * 128], pt.ap[:, :n], idx == 0, idx == len(kts) - 1,
                       [Vt.u, pt.u], [pO.u])
                    MM(pD.ap[:, :n], onesb.ap, pt.ap[:, :n], idx == 0, idx == len(kts) - 1, [onesb.u, pt.u], [pD.u])
                rd = rdp.get()
                P.add("dve", lambda e, rd=rd, pD=pD, n=n: e.reciprocal(out=rd.ap[:, :n], in_=pD.ap[:, :n]), [pD.u], [rd.u])
                TT("dve", ao.ap[:, t0:t0 + n], pO.ap[:, :n], rd.ap[:, :n], ALU.mult, [pO.u, rd.u], [ao.u])
                ps.unhold(pO)
                ps.unhold(pD)
            ST(attnT_d[h * 128:(h + 1) * 128, 0:Tq], ao.ap[:, :Tq], ao.u)
        if phase_end():
            return finish()
        sb.release(p1mark)

        def ln_rows(gd, bd):
            g_r = sb.alloc("lng", (D,), F32)
            b_r = sb.alloc("lnb", (D,), F32)
            LD(g_r.u, g_r.ap, gd[l:l + 1, :].partition_broadcast(128))
            LD(b_r.u, b_r.ap, bd[l:l + 1, :].partition_broadcast(128))
            return g_r, b_r
        lstat = Pool("lstat", 4, (8,), F32)
        junkp = Pool("junk", 1, (D,), BF16)

        def layer_norm_tile(r_ap, r_u, g_r, b_r):
            stt = lstat.get()
            P.add("dve", lambda e: e.tensor_reduce(out=stt.ap[:, 0:1], in_=r_ap, axis=AX.X, op=ALU.add), [r_u], [stt.u])
            TS("dve", stt.ap[:, 0:1], stt.ap[:, 0:1], -1.0 / D, None, ALU.mult, None, [stt.u], [stt.u])
            ACT(r_ap, r_ap, AF.Identity, [r_u, stt.u], [r_u], bias=stt.ap[:, 0:1])
            jk = junkp.get()
            ACT(jk.ap, r_ap, AF.Square, [r_u], [jk.u, stt.u], accum=stt.ap[:, 1:2])
            ACT(stt.ap[:, 1:2], stt.ap[:, 1:2], AF.Sqrt, [stt.u], [stt.u], bias=NORM_EPS, scale=1.0 / D)
            P.add("dve", lambda e: e.reciprocal(out=stt.ap[:, 1:2], in_=stt.ap[:, 1:2]), [stt.u], [stt.u])
            STT("dve", r_ap, r_ap, stt.ap[:, 1:2], g_r.ap, ALU.mult, ALU.mult, [r_u, stt.u, g_r.u], [r_u])
            TT("pool", r_ap, r_ap, b_r.ap, ALU.add, [r_u, b_r.u], [r_u])

        g_r, b_r = ln_rows(ln1g, ln1b)
        g1rows = []
        for v in range(1 if last else 2):
            t = sb.alloc("g1row%d" % v, (D,), F32)
            LD(t.u, t.ap, grow_d[l, v:v + 1, :].partition_broadcast(128))
            g1rows.append(t)
        wp = Pool("w5_", 3, (KC, CW), BF16)
        atp = Pool("attnT", 1, (KC, 512), BF16)
        cvtp = Pool("convT", 1, (KC, 512), BF16)
        gap = Pool("ga", 2, (NJ, 512), BF16)
        gcp = Pool("gc", 2, (NJ, 512), BF16)
        yT = sb.alloc("yT", (KC, 512), BF16)
        xr = sb.alloc("xr", (4, D), F32, nunits=4)
        t1p = Pool("t1", 2, (512,), F32)
        t2p = Pool("t2", 2, (512,), F32)
        for (t0, n, isc) in lblocks:
            v = 1 if isc else 0
            at = atp.get()
            LD(at.u, at.ap[:, :, :n], attnT_d[:, t0:t0 + n].rearrange("(kc p) t -> p kc t", p=128))
            cvt = cvtp.get()
            LD(cvt.u, cvt.ap[:, :, :n], convT_d[:, t0:t0 + n].rearrange("(kc p) t -> p kc t", p=128))
            for jt in range(n // 128):
                LD(xr.us[jt], xr.ap[:, jt, :], xsrc(l, t0 + jt * 128, 128))
            for cb in range(D // CW):
                wa = wload(wp, w_aob[l], cb * CW, CW, cast_u[("wao", l)])
                wc = wload(wp, w_cob[l], cb * CW, CW, cast_u[("wco", l)])
                ga = gap.get()
                LD(ga.u, ga.ap[:, :, :n], gaT_d[cb * CW:(cb + 1) * CW, t0:t0 + n].rearrange("(j p) t -> p j t", p=128))
                gc = gcp.get()
                LD(gc.u, gc.ap[:, :, :n], gcT_d[cb * CW:(cb + 1) * CW, t0:t0 + n].rearrange("(j p) t -> p j t", p=128))
                for dcl in range(NJ):
                    dc = cb * NJ + dcl
                    pA = ps.get()
                    mm_fm(pA, n, wa, dcl * 128, lambda kc: at.ap[:, kc, :n], [at.u])
                    pC = ps.get()
                    mm_fm(pC, n, wc, dcl * 128, lambda kc: cvt.ap[:, kc, :n], [cvt.u])
                    t1 = t1p.get()
                    t2 = t2p.get()
                    TT("dve", t1.ap[:, :n], pA.ap[:, :n], ga.ap[:, dcl, :n], ALU.mult, [pA.u, ga.u], [t1.u])
                    TT("dve", t2.ap[:, :n], pC.ap[:, :n], gc.ap[:, dcl, :n], ALU.mult, [pC.u, gc.u], [t2.u])
                    TT("pool", yT.ap[:, dc, :n], t1.ap[:, :n], t2.ap[:, :n], ALU.add, [t1.u, t2.u], [yT.u])
            for cb in range(D // CW):
                wm = wload(wp, w_mob[l], cb * CW, CW, cast_u[("wmo", l)])
                for jt in range(n // 128):
                    pM = ps.get()
                    for kc in range(KC):
                        MM(pM.ap[:, :CW], yT.ap[:, kc, jt * 128:(jt + 1) * 128], wm.ap[:, kc, :], kc == 0, kc == KC - 1,
                           [yT.u, wm.u], [pM.u])
                    t1 = t1p.get()
                    TT("dve", t1.ap[:, :CW], pM.ap[:, :CW], g1rows[v].ap[:, cb * CW:(cb + 1) * CW], ALU.mult,
                       [pM.u, g1rows[v].u], [t1.u])
                    xs_ = xr.ap[:, jt, cb * CW:(cb + 1) * CW]
                    STT("dve", xs_, xs_, float(cfg.alpha), t1.ap[:, :CW], ALU.mult, ALU.add, [xr.us[jt], t1.u], [xr.us[jt]])
            for jt in range(n // 128):
                layer_norm_tile(xr.ap[:, jt, :], xr.us[jt], g_r, b_r)
                ST(x1_d[t0 + jt * 128:t0 + (jt + 1) * 128, :], xr.ap[:, jt, :], xr.us[jt])
        if phase_end():
            return finish()
        sb.release(p1mark)

        g_r, b_r = ln_rows(ln2g, ln2b)
        lstat = Pool("lstat6", 4, (8,), F32)
        junkp = Pool("junk6", 1, (D,), BF16)
        g2row = sb.alloc("g2row", (D,), F32)
        rw = sb.alloc("rw", (KC, E), F32)
        LD(rw.u, rw.ap, rw_in[l].rearrange("(kc p) e -> p kc e", p=128))
        rwh = sb.alloc("rwh", (KC, E), BF16)
        rwl = sb.alloc("rwl", (KC, E), BF16)
        CP("dve", rwh.ap, rw.ap, [rw.u], [rwh.u])
        TT("dve", rwl.ap, rw.ap, rwh.ap, ALU.subtract, [rw.u, rwh.u], [rwl.u])
        vhi = sb.alloc("vhi", (KC, 128), BF16)
        vlo = sb.alloc("vlo", (KC, 128), BF16)
        bdp = Pool("bdr", 3, (CW,), F32)
        tyP = Pool("tmpy", 2, (CW,), F32)
        rbrow = sb.alloc("rbrow", (E,), F32)
        LD(rbrow.u, rbrow.ap, rb_in[l:l + 1, :].partition_broadcast(128))
        bupT = sb.alloc("bupT", (E * 2 * FC,), F32)
        nrow = E * 2 * FC
        for r0 in range(0, nrow, 128):
            rn = min(128, nrow - r0)
            load_T(bupT.ap[:, r0:r0 + rn], b_up[l].rearrange("e (j p) -> (e j) p", p=128)[r0:r0 + rn, :], rn, bupT.u)
        xpool = Pool("x6_", 1, (D,), F32)
        vT = sb.alloc("vT", (KC, 512), BF16)
        vTf = sb.alloc("vTf", (KC, 128), F32)
        acc = sb.alloc("acc", (4, D), F32, nunits=4)
        gates = sb.alloc("gates", (4, E), F32, nunits=4)
        lgp = Pool("lg", 2, (E,), F32)
        m8p = Pool("m8", 2, (8,), F32)
        mkp = Pool("mk", 2, (E,), F32)
        actp = Pool("actT", 2, (FC, 512), BF16)
        wup = Pool("wu", 2, (KC, 2, 256), BF16)
        wdp = Pool("wd", 2, (FC, CW), BF16)
        glp = Pool("glu", 2, (512,), F32)
        sgp = Pool("sig", 2, (512,), F32)
        lnp = Pool("lin", 2, (512,), F32)
        UW = min(256, F)
        NU = UW // 128
        for (t0, n, isc) in lblocks:
            v = 1 if isc else 0
            ntile = n // 128
            LD(g2row.u, g2row.ap, grow_d[l, 2 + v:3 + v, :].partition_broadcast(128))

            def router(i, col, vv):
                jt = col // 128
                pl = ps.get()
                TT("dve", vlo.ap, vTf.ap, vT.ap[:, :, col:col + 128], ALU.subtract, [vTf.u, vT.u], [vlo.u])
                trip = [(0, rwh), (0, rwl), (1, rwh)]
                for ti, (vsel, wa_) in enumerate(trip):
                    for kc in range(KC):
                        la = vT.ap[:, kc, col:col + 128] if vsel == 0 else vlo.ap[:, kc, :]
                        MM(pl.ap[:, :E], la, wa_.ap[:, kc, :], ti == 0 and kc == 0, ti == 2 and kc == KC - 1,
                           [vT.u, vlo.u, wa_.u], [pl.u])
                lg = lgp.get()
                TT("dve", lg.ap, pl.ap[:, :E], rbrow.ap, ALU.add, [pl.u, rbrow.u], [lg.u])
                m8 = m8p.get()
                P.add("dve", lambda e: e.max(out=m8.ap, in_=lg.ap), [lg.u], [m8.u])
                mk = mkp.get()
                TS("dve", mk.ap, lg.ap, m8.ap[:, 3:4], None, ALU.is_ge, None, [lg.u, m8.u], [mk.u])
                TS("dve", m8.ap[:, 0:1], m8.ap[:, 0:1], -1.0, None, ALU.mult, None, [m8.u], [m8.u])
                ACT(lg.ap, lg.ap, AF.Exp, [lg.u, m8.u], [lg.u], bias=m8.ap[:, 0:1])
                TT("dve", lg.ap, lg.ap, mk.ap, ALU.mult, [lg.u, mk.u], [lg.u])
                P.add("dve", lambda e: e.tensor_reduce(out=m8.ap[:, 1:2], in_=lg.ap, axis=AX.X, op=ALU.add), [lg.u, m8.u], [m8.u])
                P.add("dve", lambda e: e.reciprocal(out=m8.ap[:, 1:2], in_=m8.ap[:, 1:2]), [m8.u], [m8.u])
                TS("dve", gates.ap[:, jt, :], lg.ap, m8.ap[:, 1:2], None, ALU.mult, None, [lg.u, m8.u], [gates.us[jt]])
                P.add("dve", lambda e: e.memset(acc.ap[:, jt, :], 0.0), [], [acc.us[jt]])

            tiles = [(t0 // 128 + jt, jt * 128, v, vT.u) for jt in range(ntile)]
            if MOE_CUT >= 2:
                transpose_modulate(vT, lambda i: x1_d[i * 128:(i + 1) * 128, :], tiles, 3, 4, xpool, f32dst=(vTf, router))
            else:
                transpose_modulate(vT, lambda i: x1_d[i * 128:(i + 1) * 128, :], tiles, 3, 4, xpool)
                for jt in range(ntile):
                    P.add("dve", lambda e, jt=jt: e.memset(acc.ap[:, jt, :], 0.0), [], [acc.us[jt]])
            for e_ in range(E if MOE_CUT >= 3 else 0):
                actT = actp.get()
                wue = w_upb[l][e_ // EH][e_ % EH]
                for ub in range(F // UW):
                    wu = wup.get()
                    for two in range(2):
                        c0 = two * F + ub * UW
                        LD(wu.u, wu.ap[:, :, two, :UW], wue[:, c0:c0 + UW].rearrange("(kc p) f -> p kc f", p=128),
                           extra=[cast_u[("wup", l, e_ // EG)]])
                    for fl in range(NU):
                        fc = ub * NU + fl
                        pG = ps.get()
                        for kc in range(KC):
                            MM(pG.ap[:, :n], wu.ap[:, kc, 0, fl * 128:(fl + 1) * 128], vT.ap[:, kc, :n], kc == 0, kc == KC - 1,
                               [wu.u, vT.u], [pG.u])
                        pL = ps.get()
                        for kc in range(KC):
                            MM(pL.ap[:, :n], wu.ap[:, kc, 1, fl * 128:(fl + 1) * 128], vT.ap[:, kc, :n], kc == 0, kc == KC - 1,
                               [wu.u, vT.u], [pL.u])
                        bg = bupT.ap[:, e_ * 2 * FC + fc:e_ * 2 * FC + fc + 1]
                        bl = bupT.ap[:, e_ * 2 * FC + FC + fc:e_ * 2 * FC + FC + fc + 1]
                        gl = glp.get()
                        TS("dve", gl.ap[:, :n], pG.ap[:, :n], bg, SW_LIMIT, ALU.add, ALU.min, [pG.u, bupT.u], [gl.u])
                        sg = sgp.get()
                        ACT(sg.ap[:, :n], gl.ap[:, :n], AF.Sigmoid, [gl.u], [sg.u], scale=SW_ALPHA)
                        ln_ = lnp.get()
                        TS("dve", ln_.ap[:, :n], pL.ap[:, :n], bl, SW_LIMIT, ALU.add, ALU.min, [pL.u, bupT.u], [ln_.u])
                        TS("pool", ln_.ap[:, :n], ln_.ap[:, :n], -SW_LIMIT, 1.0, ALU.max, ALU.add, [ln_.u], [ln_.u])
                        TT("dve", sg.ap[:, :n], sg.ap[:, :n], gl.ap[:, :n], ALU.mult, [sg.u, gl.u], [sg.u])
                        TT("dve", actT.ap[:, fc, :n], sg.ap[:, :n], ln_.ap[:, :n], ALU.mult, [sg.u, ln_.u], [actT.u])
                wde = w_dnb[l][e_].rearrange("(fc p) d -> p fc d", p=128)
                for cb in range(D // CW if MOE_CUT >= 4 else 0):
                    wd = wdp.get()
                    LD(wd.u, wd.ap, wde[:, :, cb * CW:(cb + 1) * CW], extra=[cast_u[("wdn", l, e_ // EG)]])
                    bdr = bdp.get()
                    LD(bdr.u, bdr.ap, b_dn[l, e_:e_ + 1, cb * CW:(cb + 1) * CW].partition_broadcast(128))
                    for jt in range(ntile):
                        pY = ps.get()
                        for fc in range(FC):
                            MM(pY.ap[:, :CW], actT.ap[:, fc, jt * 128:(jt + 1) * 128], wd.ap[:, fc, :], fc == 0, fc == FC - 1,
                               [actT.u, wd.u], [pY.u])
                        a_ = acc.ap[:, jt, cb * CW:(cb + 1) * CW]
                        ty = tyP.get()
                        TT("dve", ty.ap, pY.ap[:, :CW], bdr.ap, ALU.add, [pY.u, bdr.u], [ty.u])
                        STT("dve", a_, ty.ap, gates.ap[:, jt, e_:e_ + 1], a_, ALU.mult, ALU.add,
                            [ty.u, gates.us[jt], acc.us[jt]], [acc.us[jt]])
            for jt in range(ntile):
                xt = xpool.get()
                tok = t0 + jt * 128
                LD(xt.u, xt.ap, x1_d[tok:tok + 128, :])
                a_ = acc.ap[:, jt, :]
                TT("dve", a_, a_, g2row.ap, ALU.mult, [acc.us[jt], g2row.u], [acc.us[jt]])
                STT("dve", a_, xt.ap, float(cfg.alpha), a_, ALU.mult, ALU.add, [xt.u, acc.us[jt]], [acc.us[jt]])
                layer_norm_tile(a_, acc.us[jt], g_r, b_r)
                if last:
                    ST(out_d[tok:tok + 128, :], a_, acc.us[jt])
                else:
                    ST(xs_d[tok:tok + 128, :], a_, acc.us[jt])
        if phase_end():
            return finish()
    return finish()


def rope_tables(S):
    rows = S // 64
    row = np.repeat(np.arange(rows, dtype=np.int32), 64).astype(np.float32)
    col = np.tile(np.arange(64, dtype=np.int32), rows).astype(np.float32)
    inv = (np.float32(10000.0) ** (-np.arange(0, 64, 2, dtype=np.float32) / np.float32(64))).astype(np.float32)
    ang = np.concatenate([row[:, None] * inv, col[:, None] * inv], axis=-1).astype(np.float32)
    return np.cos(ang).astype(np.float32), np.sin(ang).astype(np.float32)


def make_in_maps(cfg, inputs, ncores):
    cos, sin = rope_tables(cfg.S)
    ident = np.eye(128, dtype=np.float32)
    shared = {k: np.ascontiguousarray(np.asarray(v)) for k, v in inputs.items() if k not in ("x", "c", "ctx", "c_ctx")}
    maps = []
    for b in range(ncores):
        m = dict(shared)
        m["x"] = np.ascontiguousarray(np.asarray(inputs["x"])[b])
        m["ctx"] = np.ascontiguousarray(np.asarray(inputs["ctx"])[b])
        m["c"] = np.ascontiguousarray(np.asarray(inputs["c"])[b:b + 1])
        m["c_ctx"] = np.ascontiguousarray(np.asarray(inputs["c_ctx"])[None, :])
        m["ident"] = ident
        m["cos"] = cos
        m["sin"] = sin
        maps.append(m)
    return maps


def kernel(**inputs):
    cfg = Cfg(2048, 2048, 256, 32)
    nc = build(cfg)
    maps = make_in_maps(cfg, inputs, 8)
    res = run_bass_kernel_spmd(nc, maps, core_ids=list(range(8)))
    return np.stack([np.asarray(r["out"]) for r in res.results], axis=0).astype(np.float32)
```

```python
import numpy as np
import concourse.bass as bass
import concourse.mybir as mybir

F32 = mybir.dt.float32
BF16 = mybir.dt.bfloat16
ALU = mybir.AluOpType
AF = mybir.ActivationFunctionType
AX = mybir.AxisListType

ENGS = ["pe", "act", "dve", "pool", "sp"]
BLOCKNAME = {"pe": "tensor", "act": "scalar", "dve": "vector", "pool": "gpsimd", "sp": "sync"}
SAME_ENG_SYNC = True


class Unit:
    __slots__ = ("name", "last_w", "reads", "sem", "ndma", "base", "nobar")

    def __init__(self, name):
        self.name = name
        self.last_w = None
        self.reads = {}
        self.sem = None
        self.ndma = 0
        self.base = 0
        self.nobar = False


class Op:
    __slots__ = ("eng", "fn", "waits", "is_dma", "unit", "signal", "count", "inc16", "sem")

    def __init__(self, eng, fn, is_dma=False, unit=None):
        self.eng = eng
        self.fn = fn
        self.waits = []
        self.is_dma = is_dma
        self.unit = unit
        self.signal = False
        self.count = 0
        self.inc16 = False


class Prog:
    def __init__(self, nc, stack):
        self.nc = nc
        self.stack = stack
        self.ops = []
        self.units = []
        self.last_c = {e: None for e in ENGS}
        self.esem = {}
        for e in ["pe", "act", "dve", "pool"]:
            self.esem[e] = stack.enter_context(nc.semaphore("es_" + e))
        self.nsem = 4
        self.free_sems = []

    def recycle(self, units):
        for u in units:
            if u.sem is not None:
                self.free_sems.append((u.sem, u.base + 16 * u.ndma))
                u.sem = None
        dead = set(id(u) for u in units)
        self.units = [u for u in self.units if id(u) not in dead]

    def unit(self, name):
        u = Unit(name)
        self.units.append(u)
        return u

    def _sem_for(self, u, fresh=False):
        if u.sem is None:
            if self.free_sems and not fresh:
                u.sem, u.base = self.free_sems.pop(0)
            else:
                u.sem = self.stack.enter_context(self.nc.semaphore("us_%d" % self.nsem))
                self.nsem += 1
                u.base = 0
        return u.sem

    def _dep(self, op, prod, war=False):
        if prod.is_dma:
            return
        if prod.eng == op.eng:
            if op.eng == "pe" or war or not SAME_ENG_SYNC:
                return
        prod.signal = True
        op.waits.append(("op", prod))

    def _dma_wait(self, op, u):
        if u.ndma > 0:
            op.waits.append(("dma", u.sem, u.base + 16 * u.ndma))

    def add(self, eng, fn, reads=(), writes=()):
        op = Op(eng, fn)
        for u in reads:
            lw = u.last_w
            if lw is not None:
                if lw.is_dma:
                    self._dma_wait(op, u)
                else:
                    self._dep(op, lw)
        for u in writes:
            lw = u.last_w
            if lw is not None and not lw.is_dma:
                self._dep(op, lw)
            self._dma_wait(op, u)
            for e, r in u.reads.items():
                self._dep(op, r, war=True)
        for u in reads:
            u.reads[eng] = op
        for u in writes:
            u.last_w = op
            u.reads = {}
        self.ops.append(op)
        self.last_c[eng] = op
        return op

    def dma(self, q, out, in_, unit, load, extra_reads=(), **kw):
        op = Op(q, lambda e: e.dma_start(out=out, in_=in_, **kw), is_dma=True, unit=unit)
        u = unit
        op.sem = self._sem_for(u)
        if load:
            lw = u.last_w
            if lw is not None and not lw.is_dma:
                self._dep(op, lw)
            self._dma_wait(op, u)
            for e, r in u.reads.items():
                self._dep(op, r)
            u.last_w = op
            u.reads = {}
        else:
            lw = u.last_w
            if lw is not None:
                if lw.is_dma:
                    self._dma_wait(op, u)
                else:
                    self._dep(op, lw)
        for u2 in extra_reads:
            lw = u2.last_w
            if lw is not None:
                if lw.is_dma:
                    self._dma_wait(op, u2)
                else:
                    self._dep(op, lw)
        u.ndma += 1
        self.ops.append(op)
        return op

    def raw16(self, q, fn, unit):
        op = Op(q, fn, is_dma=True, unit=unit)
        op.sem = self._sem_for(unit, fresh=True)
        self._dma_wait(op, unit)
        unit.last_w = op
        unit.ndma += 1
        self.ops.append(op)
        return op

    def wait_unit(self, eng, u):
        op = Op(eng, None)
        self._dma_wait(op, u)
        self.ops.append(op)

    def barrier(self, full=False):
        snap = dict(self.last_c)
        for e, p in snap.items():
            if p is not None:
                p.signal = True
        dmas = [(u.sem, u.base + 16 * u.ndma) for u in self.units if u.ndma > 0 and u.sem is not None and (full or not u.nobar)]
        for e in ENGS:
            op = Op(e, None)
            for e2, p in snap.items():
                if p is not None and e2 != e:
                    op.waits.append(("op", p))
            for u, v in dmas:
                op.waits.append(("dma", u, v))
            self.ops.append(op)

    def emit(self):
        cnt = {e: 0 for e in ENGS}
        for op in self.ops:
            if op.signal and not op.is_dma:
                cnt[op.eng] += 1
                op.count = cnt[op.eng]
        nc = self.nc
        stats = {e: [0, 0] for e in ENGS}
        with nc.Block() as block:
            for e in ENGS:
                def body(engobj, e=e):
                    seen = {}
                    for op in self.ops:
                        if op.eng != e:
                            continue
                        for w in op.waits:
                            if w[0] == "op":
                                sem, val, key = self.esem[w[1].eng], w[1].count, w[1].eng
                            else:
                                sem, val, key = w[1], w[2], id(w[1])
                            if seen.get(key, 0) >= val:
                                continue
                            engobj.wait_ge(sem, val)
                            seen[key] = val
                            stats[e][1] += 1
                        if op.fn is not None:
                            inst = op.fn(engobj)
                            stats[e][0] += 1
                            if op.is_dma:
                                inst.then_inc(op.sem, 16)
                            elif op.signal:
                                inst.then_inc(self.esem[e], 1)
                getattr(block, BLOCKNAME[e])(body)
        return stats, cnt


class SB:
    def __init__(self, prog, nwords):
        self.p = prog
        self.nw = nwords
        self.big = prog.stack.enter_context(prog.nc.sbuf_tensor("big", [128, nwords], F32))
        self.off = 0
        self.peak = 0
        self.allocs = []

    def mark(self):
        return self.off

    def release(self, m):
        dead = []
        while self.allocs and self.allocs[-1][0] >= m:
            dead.extend(self.allocs.pop()[1].us)
        self.p.recycle(dead)
        self.off = m

    def alloc(self, name, free_shape, dtype=F32, nunits=1):
        n = int(np.prod(free_shape))
        esz = 4 if dtype == F32 else 2
        words = (n * esz + 3) // 4
        words = (words + 7) // 8 * 8
        assert self.off + words <= self.nw, "SBUF overflow %s %d+%d>%d" % (name, self.off, words, self.nw)
        ap = self.big[:, self.off:self.off + words]
        if dtype != F32:
            ap = ap.bitcast(dtype)
        ap = ap[:, 0:n]
        if len(free_shape) == 2:
            ap = ap.rearrange("p (a b) -> p a b", b=free_shape[1])
        elif len(free_shape) == 3:
            ap = ap.rearrange("p (a b c) -> p a b c", b=free_shape[1], c=free_shape[2])
        self.off += words
        self.peak = max(self.peak, self.off)
        t = T(ap, [self.p.unit("%s.%d" % (name, i)) for i in range(nunits)])
        self.allocs.append((self.off - words, t))
        return t


class T:
    __slots__ = ("ap", "us")

    def __init__(self, ap, us):
        self.ap = ap
        self.us = us

    @property
    def u(self):
        return self.us[0]


class PS:
    def __init__(self, prog):
        self.p = prog
        self.banks = []
        for i in range(8):
            t = prog.stack.enter_context(prog.nc.psum_tensor("psb%d" % i, [128, 512], F32))
            self.banks.append(T(t[:, :], [prog.unit("ps%d" % i)]))
        self.i = 0

        self.rot = list(self.banks)

    def get(self):
        b = self.rot[self.i % len(self.rot)]
        self.i += 1
        return b

    def hold(self):
        b = self.rot.pop(self.i % len(self.rot))
        return b

    def unhold(self, b):
        self.rot.append(b)

from contextlib import ExitStack
from concourse.bass_utils import run_bass_kernel_spmd

NORM_EPS = 1e-6
SW_ALPHA = 1.702
SW_LIMIT = 7.0


class Cfg:
    def __init__(self, D, S, L, E, DEPTH=2):
        self.D, self.S, self.L, self.E, self.DEPTH = D, S, L, E, DEPTH
        self.KC = D // 128
        self.H = self.KC
        self.HKV = self.H // 4
        self.DKV = self.HKV * 128
        self.F = D // 2
        self.FC = self.F // 128
        self.NIN = 6 * D + 2 * self.DKV
        self.T = S + L
        self.NT = self.T // 128
        self.NTL = S // 128
        self.CW = min(512, D)
        self.O_K = D
        self.O_V = D + self.DKV
        self.O_CB = D + 2 * self.DKV
        self.O_CC = self.O_CB + D
        self.O_CX = self.O_CC + D
        self.O_GA = self.O_CX + D
        self.O_GC = self.O_GA + D
        self.blocks = []
        for t0 in range(0, S, 512):
            self.blocks.append((t0, min(512, S - t0), False))
        for t0 in range(S, self.T, 512):
            self.blocks.append((t0, min(512, self.T - t0), True))
        self.alpha = (2 * DEPTH) ** 0.25


def build(cfg, nphase=99, debug=()):
    import os
    MOE_CUT = int(os.environ.get('MOE_CUT', '9'))
    D, S, L, E, DEPTH = cfg.D, cfg.S, cfg.L, cfg.E, cfg.DEPTH
    KC, H, HKV, DKV, F, FC, NIN, T, NT, NTL, CW = (cfg.KC, cfg.H, cfg.HKV, cfg.DKV, cfg.F, cfg.FC,
                                                   cfg.NIN, cfg.T, cfg.NT, cfg.NTL, cfg.CW)
    NJ = CW // 128
    blocks = cfg.blocks
    nc = bass.Bass("TRN2", target_bir_lowering=False)

    def din(name, shape, dt=F32):
        return nc.dram_tensor(name, list(shape), dt, kind="ExternalInput").ap()

    def dint(name, shape, dt):
        return nc.dram_tensor(name, list(shape), dt).ap()

    x_in = din("x", [S, D])
    ctx_in = din("ctx", [L, D])
    c_in = din("c", [1, D])
    cctx_in = din("c_ctx", [1, D])
    ada_w = din("ada_w", [DEPTH, D, 6 * D])
    ada_b = din("ada_b", [DEPTH, 6 * D])
    w_in = din("w_in", [DEPTH, D, NIN])
    qg_in = din("q_norm_g", [DEPTH, 128])
    kg_in = din("k_norm_g", [DEPTH, 128])
    convw_in = din("conv_w", [DEPTH, 3, D])
    w_ao = din("w_attn_o", [DEPTH, D, D])
    w_co = din("w_conv_o", [DEPTH, D, D])
    w_mo = din("w_mix_o", [DEPTH, D, D])
    ln1g = din("ln1_g", [DEPTH, D])
    ln1b = din("ln1_b", [DEPTH, D])
    rw_in = din("router_w", [DEPTH, D, E])
    rb_in = din("router_b", [DEPTH, E])
    w_up = din("w_up", [DEPTH, E, D, 2 * F])
    b_up = din("b_up", [DEPTH, E, 2 * F])
    w_dn = din("w_down", [DEPTH, E, F, D])
    b_dn = din("b_down", [DEPTH, E, D])
    ln2g = din("ln2_g", [DEPTH, D])
    ln2b = din("ln2_b", [DEPTH, D])
    ident_in = din("ident", [128, 128])
    cos_in = din("cos", [S, 64])
    sin_in = din("sin", [S, 64])
    out_d = nc.dram_tensor("out", [S, D], F32, kind="ExternalOutput").ap()

    ada_wb = dint("ada_wb", [DEPTH, D, 6 * D], BF16)
    w_inb = dint("w_inb", [DEPTH, D, NIN], BF16)
    w_aob = dint("w_aob", [DEPTH, D, D], BF16)
    w_cob = dint("w_cob", [DEPTH, D, D], BF16)
    w_mob = dint("w_mob", [DEPTH, D, D], BF16)
    EH = E // 2
    w_upb = [[dint("w_upb_%d_%d" % (l_, h_), [EH, D, 2 * F], BF16) for h_ in range(2)] for l_ in range(DEPTH)]
    w_dnb = [dint("w_dnb_%d" % l_, [E, F, D], BF16) for l_ in range(DEPTH)]
    qT_d = dint("qT_d", [D, T], BF16)
    kT_d = dint("kT_d", [DKV, T], BF16)
    V_d = dint("V_d", [T, DKV], BF16)
    convT_d = dint("convT_d", [D, T], BF16)
    gaT_d = dint("gaT_d", [D, T], BF16)
    gcT_d = dint("gcT_d", [D, T], BF16)
    attnT_d = dint("attnT_d", [D, T], BF16)
    x1_d = dint("x1_d", [T, D], F32)
    xs_d = dint("xs_d", [T, D], F32)
    grow_d = dint("grow_d", [DEPTH, 4, D], F32)

    stack = ExitStack()
    P = Prog(nc, stack)
    sb = SB(P, 51800)
    ps = PS(P)

    def TT(eng, out, in0, in1, op, R, W):
        P.add(eng, lambda e: e.tensor_tensor(out=out, in0=in0, in1=in1, op=op), R, W)

    def TS(eng, out, in0, s1, s2, op0, op1, R, W):
        if op1 is None:
            P.add(eng, lambda e: e.tensor_scalar(out=out, in0=in0, scalar1=s1, scalar2=None, op0=op0), R, W)
        else:
            P.add(eng, lambda e: e.tensor_scalar(out=out, in0=in0, scalar1=s1, scalar2=s2, op0=op0, op1=op1), R, W)

    def STT(eng, out, in0, scalar, in1, op0, op1, R, W):
        P.add(eng, lambda e: e.scalar_tensor_tensor(out=out, in0=in0, scalar=scalar, in1=in1, op0=op0, op1=op1), R, W)

    def ACT(out, in_, func, R, W, bias=0.0, scale=1.0, accum=None):
        if accum is None:
            P.add("act", lambda e: e.activation(out=out, in_=in_, func=func, bias=bias, scale=scale), R, W)
        else:
            P.add("act", lambda e: e.activation(out=out, in_=in_, func=func, bias=bias, scale=scale, accum_out=accum), R, W)

    def CP(eng, out, in_, R, W):
        if eng == "act":
            P.add("act", lambda e: e.copy(out=out, in_=in_), R, W)
        else:
            P.add(eng, lambda e: e.tensor_copy(out=out, in_=in_), R, W)

    def MM(out, lhsT, rhs, start, stop, R, W):
        P.add("pe", lambda e: e.matmul(out, lhsT=lhsT, rhs=rhs, start=start, stop=stop), R, W)

    def TR(out, in_, ident, R, W):
        P.add("pe", lambda e: e.transpose(out=out, in_=in_, identity=ident), R, W)

    def LD(dst_t, dst_ap, src, extra=(), q="sp"):
        P.dma(q, dst_ap, src, dst_t, True, extra_reads=extra)

    def ST(dst, src_ap, src_u, q="act"):
        P.dma(q, dst, src_ap, src_u, False)

    class Pool:
        def __init__(self, name, n, shape, dt):
            self.ts = [sb.alloc("%s%d" % (name, i), shape, dt) for i in range(n)]
            self.i = 0

        def get(self):
            t = self.ts[self.i % len(self.ts)]
            self.i += 1
            return t

    rr = {"n": 0}

    def alt(engs):
        rr["n"] += 1
        return engs[rr["n"] % len(engs)]

    cast_u = {}

    def cast(name, l, src2d, dst2d, rows, rchunk):
        u = P.unit("cast_%s%d" % (name, l))
        u.nobar = True
        cast_u[(name, l)] = u
        for r0 in range(0, rows, rchunk):
            r1 = min(rows, r0 + rchunk)
            P.raw16("pool", lambda e, r0=r0, r1=r1: e.dma_start(out=dst2d[r0:r1, :], in_=src2d[r0:r1, :]), u)

    NG = 8
    EG = E // NG

    def cast_rows(u, src2d, dst2d, rows, rchunk):
        for r0 in range(0, rows, rchunk):
            r1 = min(rows, r0 + rchunk)
            P.raw16("pool", lambda e, r0=r0, r1=r1: e.dma_start(out=dst2d[r0:r1, :], in_=src2d[r0:r1, :]), u)

    def issue_casts():
        for l in range(DEPTH):
            if l > 0:
                cast("ada", l, ada_w[l], ada_wb[l], D, 128)
                cast("win", l, w_in[l], w_inb[l], D, 128)
            cast("wao", l, w_ao[l], w_aob[l], D, 512)
            cast("wco", l, w_co[l], w_cob[l], D, 512)
            cast("wmo", l, w_mo[l], w_mob[l], D, 512)
            for g_ in range(NG):
                e0 = g_ * EG
                hh, i0 = e0 // EH, e0 % EH
                u = P.unit("cast_wup%d_%d" % (l, g_))
                u.nobar = True
                cast_u[("wup", l, g_)] = u
                cast_rows(u, w_up[l, e0:e0 + EG].rearrange("e d f -> (e d) f"),
                          w_upb[l][hh][i0:i0 + EG].rearrange("e d f -> (e d) f"), EG * D, 512)
                u = P.unit("cast_wdn%d_%d" % (l, g_))
                u.nobar = True
                cast_u[("wdn", l, g_)] = u
                cast_rows(u, w_dn[l, e0:e0 + EG].rearrange("e f d -> (e f) d"),
                          w_dnb[l][e0:e0 + EG].rearrange("e f d -> (e f) d"), EG * F, 512)

    ident = sb.alloc("ident", (128,), F32)
    LD(ident.u, ident.ap, ident_in)
    identb = sb.alloc("identb", (128,), BF16)
    CP("dve", identb.ap, ident.ap, [ident.u], [identb.u])
    onesf = sb.alloc("onesf", (128,), F32)
    P.add("dve", lambda e: e.memset(onesf.ap, 1.0), [], [onesf.u])
    onesb = sb.alloc("onesb", (128,), BF16)
    P.add("dve", lambda e: e.memset(onesb.ap, 1.0), [], [onesb.u])

    def load_T(dst_ap, rows_ap, R, dst_u, eng="dve"):
        m = sb.mark()
        tmp = sb.alloc("ldT", (128,), F32)
        LD(tmp.u, tmp.ap[:R, :], rows_ap)
        b = ps.get()
        TR(b.ap[:, 0:R], tmp.ap[:R, :], ident.ap[:R, :R], [tmp.u, ident.u], [b.u])
        CP(eng, dst_ap, b.ap[:, 0:R], [b.u], [dst_u])
        return m

    silT = sb.alloc("silT", (2, KC), F32)
    for v, src in ((0, c_in), (1, cctx_in)):
        tmp = sb.alloc("ctmp%d" % v, (128,), F32)
        LD(tmp.u, tmp.ap[:KC, :], src.rearrange("o (kc p) -> (o kc) p", p=128))
        b = ps.get()
        TR(b.ap[:, 0:KC], tmp.ap[:KC, :], ident.ap[:KC, :KC], [tmp.u, ident.u], [b.u])
        ACT(silT.ap[:, v, :], b.ap[:, 0:KC], AF.Silu, [b.u], [silT.u])
    sil = sb.alloc("sil", (KC, 2), BF16)
    for v in range(2):
        CP("dve", sil.ap[:, :, v], silT.ap[:, v, :], [silT.u], [sil.u])
    base_mark = sb.mark()

    def xsrc(l, t0, n):
        if l == 0:
            if t0 < S:
                return x_in[t0:t0 + n, :]
            return ctx_in[t0 - S:t0 - S + n, :]
        return xs_d[t0:t0 + n, :]

    stage_ref = {}

    def wload(pool, w2d, c0, cw, cu, direct=None):
        t = pool.get()
        if direct is None:
            LD(t.u, t.ap[:, :, :cw], w2d[:, c0:c0 + cw].rearrange("(kc p) c -> p kc c", p=128), extra=[cu])
        else:
            KH = KC // 4
            for hf in range(4):
                st = stage_ref["p"].get()
                k0 = hf * KH
                LD(st.u, st.ap[:, :, :cw],
                   direct[k0 * 128:(k0 + KH) * 128, c0:c0 + cw].rearrange("(kc p) c -> p kc c", p=128))
                CP(alt(["act", "pool"]), t.ap[:, k0:k0 + KH, :cw], st.ap[:, :, :cw], [st.u], [t.u])
        return t

    def mm_fm(pb, n, wt, c0, rhs_ap_fn, units):
        for kc in range(KC):
            MM(pb.ap[:, :n], wt.ap[:, kc, c0:c0 + 128], rhs_ap_fn(kc), kc == 0, kc == KC - 1, [wt.u] + units, [pb.u])

    phase = {"n": 0}

    def phase_end():
        P.barrier()
        phase["n"] += 1
        return phase["n"] >= nphase

    scratch = {"qT_d": qT_d, "kT_d": kT_d, "V_d": V_d, "convT_d": convT_d, "gaT_d": gaT_d, "gcT_d": gcT_d,
               "attnT_d": attnT_d, "x1_d": x1_d, "xs_d": xs_d, "grow_d": grow_d}

    def finish():
        P.barrier(full=True)
        for name in debug:
            src = scratch[name]
            dst = nc.dram_tensor("dbg_" + name, list(src.shape), F32, kind="ExternalOutput").ap()
            du = P.unit("dbg_" + name)
            if len(src.shape) == 3:
                for i in range(src.shape[0]):
                    P.raw16("pool", lambda e, i=i, dst=dst, src=src: e.dma_start(out=dst[i], in_=src[i]), du)
            else:
                P.raw16("pool", lambda e, dst=dst, src=src: e.dma_start(out=dst, in_=src), du)
        P.barrier(full=True)
        st = P.emit()
        print("emit stats", st, "sbuf peak words", sb.peak, "nsem", P.nsem, flush=True)
        stack.close()
        return nc

    for l in range(DEPTH):
        last = (l == DEPTH - 1)
        lblocks = [b for b in blocks if not (last and b[2])]
        sb.release(base_mark)
        modA = sb.alloc("modA", (6 * KC, 2), F32)
        p1mark = sb.mark()
        silrep = sb.alloc("silrep", (2, KC, 128), BF16)
        for v in range(2):
            for kc in range(KC):
                TS("dve", silrep.ap[:, v, kc, :], onesf.ap, silT.ap[:, v, kc:kc + 1], None, ALU.mult, None,
                   [onesf.u, silT.u], [silrep.u])
        adabT = sb.alloc("adabT", (6 * KC,), F32)
        load_T(adabT.ap, ada_b[l].rearrange("(j p) -> j p", p=128), 6 * KC, adabT.u)
        wp = Pool("w1_", 3, (KC, CW), BF16)
        if l == 0:
            stage_ref["p"] = Pool("stg1_", 2, (KC // 4, CW), F32)
        growp = Pool("grow", 2, (CW,), F32)
        abp = Pool("abrow", 2, (CW,), F32)
        modps = ps.hold()
        for b in range(6 * D // CW):
            wt = wload(wp, ada_wb[l], b * CW, CW, cast_u.get(("ada", l)), direct=(ada_w[l] if l == 0 else None))
            for jl in range(NJ):
                j = b * NJ + jl
                for kc in range(KC):
                    MM(modps.ap[:, 2 * j:2 * j + 2], wt.ap[:, kc, jl * 128:(jl + 1) * 128], sil.ap[:, kc, :],
                       kc == 0, kc == KC - 1, [wt.u, sil.u], [modps.u])
            sec = (b * CW) // D
            if sec in (2, 5):
                for v in range(2):
                    if last and v == 1:
                        continue
                    pb = ps.get()
                    for kc in range(KC):
                        MM(pb.ap[:, 0:CW], silrep.ap[:, v, kc, :], wt.ap[:, kc, :], kc == 0, kc == KC - 1,
                           [wt.u, silrep.u], [pb.u])
                    ab = abp.get()
                    LD(ab.u, ab.ap[0:1, :], ada_b[l:l + 1, b * CW:(b + 1) * CW])
                    gr = growp.get()
                    TT("dve", gr.ap[0:1, :], pb.ap[0:1, 0:CW], ab.ap[0:1, :], ALU.add, [pb.u, ab.u], [gr.u])
                    c0 = b * CW - sec * D
                    ST(grow_d[l, (2 if sec == 5 else 0) + v:(2 if sec == 5 else 0) + v + 1, c0:c0 + CW], gr.ap[0:1, :], gr.u)
        mview = modps.ap[:, 0:12 * KC].rearrange("p (j v) -> p j v", v=2)
        for v in range(2):
            TT("dve", modA.ap[:, :, v], mview[:, :, v], adabT.ap, ALU.add, [modps.u, adabT.u], [modA.u])
        for sec in (1, 4):
            TS("dve", modA.ap[:, sec * KC:(sec + 1) * KC, :], modA.ap[:, sec * KC:(sec + 1) * KC, :], 1.0, None,
               ALU.add, None, [modA.u], [modA.u])
        ps.unhold(modps)
        if phase_end():
            return finish()
        sb.release(p1mark)

        def mod_ap(sec, kc, v):
            return modA.ap[:, sec * KC + kc, v:v + 1]

        def transpose_modulate(dstT, src_rows_fn, tiles, sec_sh, sec_sc, xpool, f32dst=None):
            for (i, col, v, du) in tiles:
                xt = xpool.get()
                LD(xt.u, xt.ap, src_rows_fn(i))
                for kg in range(0, KC, 4):
                    b = ps.get()
                    nk = min(4, KC - kg)
                    for q in range(nk):
                        TR(b.ap[:, q * 128:(q + 1) * 128], xt.ap[:, (kg + q) * 128:(kg + q + 1) * 128], ident.ap,
                           [xt.u, ident.u], [b.u])
                    for q in range(nk):
                        kc = kg + q
                        eng = alt(["act", "dve"])
                        o = dstT.ap[:, kc, col:col + 128]
                        src = b.ap[:, q * 128:(q + 1) * 128]
                        if f32dst is not None:
                            tgt, tu = f32dst[0].ap[:, kc, :], f32dst[0].u
                        else:
                            tgt, tu = o, du
                        if eng == "act":
                            ACT(tgt, src, AF.Identity, [b.u, modA.u], [tu],
                                bias=mod_ap(sec_sh, kc, v), scale=mod_ap(sec_sc, kc, v))
                        else:
                            TS("dve", tgt, src, mod_ap(sec_sc, kc, v), mod_ap(sec_sh, kc, v),
                               ALU.mult, ALU.add, [b.u, modA.u], [tu])
                        if f32dst is not None:
                            CP("pool", o, tgt, [tu], [du])
                if f32dst is not None:
                    f32dst[1](i, col, v)

        p2mark = sb.mark()
        uT = sb.alloc("uT", (KC, T), BF16, nunits=len(blocks))

        def blk_of(tok):
            for bi, (t0, n, isc) in enumerate(blocks):
                if t0 <= tok < t0 + n:
                    return bi
        xpool = Pool("xt", 2, (D,), F32)
        tiles = [(i, i * 128, 1 if i >= NTL else 0, uT.us[blk_of(i * 128)]) for i in range(NT)]
        transpose_modulate(uT, lambda i: xsrc(l, i * 128, 128), tiles, 0, 1, xpool)

        wp = Pool("w3_", 3, (KC, CW), BF16)
        wcu = cast_u.get(("win", l))
        if l == 0:
            stage_ref["p"] = Pool("stg3_", 2, (KC // 4, CW), F32)

        def wl3(c0, cw):
            return wload(wp, w_inb[l], c0, cw, wcu, direct=(w_in[l] if l == 0 else None))
        m3 = sb.mark()
        cosT = sb.alloc("cosT", (NTL, 64), F32)
        sinT = sb.alloc("sinT", (NTL, 64), F32)
        LD(cosT.u, cosT.ap, cos_in.rearrange("(i p) f -> p i f", p=128))
        LD(sinT.u, sinT.ap, sin_in.rearrange("(i p) f -> p i f", p=128))
        grow_q = sb.alloc("grow_q", (128,), F32)
        grow_k = sb.alloc("grow_k", (128,), F32)
        LD(grow_q.u, grow_q.ap, qg_in[l:l + 1, :].partition_broadcast(128))
        LD(grow_k.u, grow_k.ap, kg_in[l:l + 1, :].partition_broadcast(128))
        TS("dve", grow_q.ap, grow_q.ap, float(128 ** -0.5), None, ALU.mult, None, [grow_q.u], [grow_q.u])
        sqp = Pool("sq", 2, (CW,), F32)
        xnp = Pool("xn", 2, (CW,), F32)
        ssp = Pool("ss", 2, (8,), F32)
        rp_a = Pool("rpa", 2, (CW // 2,), F32)
        rp_b = Pool("rpb", 2, (CW // 2,), F32)
        qnp = Pool("qn", 2, (CW,), BF16)
        stgp = Pool("qstg", 2, (NJ, 512), BF16)

        def qk_chain(pb, cw, i, grow, rope):
            nh = cw // 128
            sq = sqp.get()
            ACT(sq.ap[:, :cw], pb.ap[:, :cw], AF.Square, [pb.u], [sq.u])
            ss = ssp.get()
            P.add("dve", lambda e: e.tensor_reduce(out=ss.ap[:, :nh], in_=sq.ap[:, :cw].rearrange("p (h d) -> p h d", d=128),
                                                   axis=AX.X, op=ALU.add), [sq.u], [ss.u])
            ACT(ss.ap[:, :nh], ss.ap[:, :nh], AF.Sqrt, [ss.u], [ss.u], bias=NORM_EPS, scale=1.0 / 128)
            P.add("dve", lambda e: e.reciprocal(out=ss.ap[:, :nh], in_=ss.ap[:, :nh]), [ss.u], [ss.u])
            xn = xnp.get()
            for hh in range(nh):
                STT("dve", xn.ap[:, hh * 128:(hh + 1) * 128], pb.ap[:, hh * 128:(hh + 1) * 128], ss.ap[:, hh:hh + 1],
                    grow.ap, ALU.mult, ALU.mult, [pb.u, ss.u, grow.u], [xn.u])
            qn = qnp.get()
            if not rope:
                CP("act", qn.ap[:, :cw], xn.ap[:, :cw], [xn.u], [qn.u])
                return qn
            xv = xn.ap[:, :cw].rearrange("p (h f two) -> p h f two", two=2, f=64)
            qv = qn.ap[:, :cw].rearrange("p (h f two) -> p h f two", two=2, f=64)
            ta = rp_a.get()
            tb = rp_b.get()
            for hh in range(nh):
                x0 = xv[:, hh, :, 0]
                x1 = xv[:, hh, :, 1]
                a = ta.ap[:, hh * 64:(hh + 1) * 64]
                bq = tb.ap[:, hh * 64:(hh + 1) * 64]
                cs = cosT.ap[:, i, :]
                sn = sinT.ap[:, i, :]
                TT("dve", a, x0, cs, ALU.mult, [xn.u, cosT.u], [ta.u])
                TT("pool", bq, x1, sn, ALU.mult, [xn.u, sinT.u], [tb.u])
                TT("dve", qv[:, hh, :, 0], a, bq, ALU.subtract, [ta.u, tb.u], [qn.u])
                TT("pool", a, x0, sn, ALU.mult, [xn.u, sinT.u, qn.u], [ta.u])
                TT("dve", bq, x1, cs, ALU.mult, [xn.u, cosT.u, qn.u], [tb.u])
                TT("pool", qv[:, hh, :, 1], a, bq, ALU.add, [ta.u, tb.u], [qn.u])
            return qn

        def qk_group(c0, ncols, grow, dstT_d, is_q):
            for cb0 in range(0, ncols, CW):
                cw = min(CW, ncols - cb0)
                nh = cw // 128
                wt = wl3(c0 + cb0, cw)
                for bi, (t0, n, isc) in enumerate(blocks):
                    stg = stgp.get()
                    for jt in range(n // 128):
                        i = t0 // 128 + jt
                        pb = ps.get()
                        for kc in range(KC):
                            MM(pb.ap[:, :cw], uT.ap[:, kc, i * 128:(i + 1) * 128], wt.ap[:, kc, :cw], kc == 0, kc == KC - 1,
                               [uT.us[bi], wt.u], [pb.u])
                        qn = qk_chain(pb, cw, i, grow, rope=not isc)
                        pt = ps.get()
                        ptb = pt.ap.bitcast(BF16)
                        for hh in range(nh):
                            TR(ptb[:, hh * 128:(hh + 1) * 128], qn.ap[:, hh * 128:(hh + 1) * 128], identb.ap,
                               [qn.u, identb.u], [pt.u])
                        CP(alt(["act", "dve"]), stg.ap[:, :nh, jt * 128:(jt + 1) * 128],
                           ptb[:, 0:cw].rearrange("p (h t) -> p h t", t=128), [pt.u], [stg.u])
                    ST(dstT_d[cb0:cb0 + cw, t0:t0 + n].rearrange("(h p) t -> p h t", p=128), stg.ap[:, :nh, :n], stg.u)

        qk_group(0, D, grow_q, qT_d, True)
        qk_group(cfg.O_K, DKV, grow_k, kT_d, False)
        vstp = Pool("vst", 2, (CW,), BF16)
        for cb0 in range(0, DKV, CW):
            cw = min(CW, DKV - cb0)
            wt = wl3(cfg.O_V + cb0, cw)
            for bi, (t0, n, isc) in enumerate(blocks):
                for jt in range(n // 128):
                    i = t0 // 128 + jt
                    pb = ps.get()
                    for kc in range(KC):
                        MM(pb.ap[:, :cw], uT.ap[:, kc, i * 128:(i + 1) * 128], wt.ap[:, kc, :cw], kc == 0, kc == KC - 1,
                           [uT.us[bi], wt.u], [pb.u])
                    vs = vstp.get()
                    CP(alt(["act", "dve"]), vs.ap[:, :cw], pb.ap[:, :cw], [pb.u], [vs.u])
                    ST(V_d[i * 128:(i + 1) * 128, cb0:cb0 + cw], vs.ap[:, :cw], vs.u)
        P.barrier()
        sb.release(m3)
        W = T + 3

        def zcol(t):
            return t + 1 if t < S else t + 2
        cwT = sb.alloc("cwT", (3 * KC,), F32)
        load_T(cwT.ap, convw_in[l].rearrange("j (kc p) -> (j kc) p", p=128), 3 * KC, cwT.u)
        zp = Pool("z", 2, (W,), F32)
        for zt in zp.ts:
            P.add("dve", lambda e, zt=zt: e.memset(zt.ap, 0.0), [], [zt.u])
        cvp = Pool("cv", 1, (W,), F32)
        csp = Pool("cS", 2, (512,), F32)
        cstp = Pool("cst", 2, (T,), BF16)
        for cb in range(D // CW):
            wC = wl3(cfg.O_CC + cb * CW, CW)
            wX = wl3(cfg.O_CX + cb * CW, CW)
            wB = wl3(cfg.O_CB + cb * CW, CW)
            for dcl in range(NJ):
                dc = cb * NJ + dcl
                z = zp.get()
                for bi, (t0, n, isc) in enumerate(blocks):
                    pC = ps.get()
                    mm_fm(pC, n, wC, dcl * 128, lambda kc: uT.ap[:, kc, t0:t0 + n], [uT.us[bi]])
                    pX = ps.get()
                    mm_fm(pX, n, wX, dcl * 128, lambda kc: uT.ap[:, kc, t0:t0 + n], [uT.us[bi]])
                    cS = csp.get()
                    CP("act", cS.ap[:, :n], pC.ap[:, :n], [pC.u], [cS.u])
                    TT("dve", z.ap[:, zcol(t0):zcol(t0) + n], pX.ap[:, :n], cS.ap[:, :n], ALU.mult, [pX.u, cS.u], [z.u])
                cv = cvp.get()
                ACT(cv.ap[:, 1:W - 1], z.ap[:, 1:W - 1], AF.Identity, [z.u, cwT.u], [cv.u], scale=cwT.ap[:, KC + dc:KC + dc + 1])
                STT("dve", cv.ap[:, 1:W - 1], z.ap[:, 0:W - 2], cwT.ap[:, dc:dc + 1], cv.ap[:, 1:W - 1], ALU.mult, ALU.add,
                    [z.u, cwT.u, cv.u], [cv.u])
                STT("dve", cv.ap[:, 1:W - 1], z.ap[:, 2:W], cwT.ap[:, 2 * KC + dc:2 * KC + dc + 1], cv.ap[:, 1:W - 1],
                    ALU.mult, ALU.add, [z.u, cwT.u, cv.u], [cv.u])
                cst = cstp.get()
                for bi, (t0, n, isc) in enumerate(blocks):
                    pB = ps.get()
                    mm_fm(pB, n, wB, dcl * 128, lambda kc: uT.ap[:, kc, t0:t0 + n], [uT.us[bi]])
                    TT("dve", cst.ap[:, t0:t0 + n], pB.ap[:, :n], cv.ap[:, zcol(t0):zcol(t0) + n], ALU.mult, [pB.u, cv.u], [cst.u])
                ST(convT_d[dc * 128:(dc + 1) * 128, :], cst.ap, cst.u)
        P.barrier()
        sb.release(m3)
        gstp = Pool("gst", 2, (T,), BF16)
        for (og, gd) in ((cfg.O_GA, gaT_d), (cfg.O_GC, gcT_d)):
            for cb in range(D // CW):
                wt = wl3(og + cb * CW, CW)
                for dcl in range(NJ):
                    dc = cb * NJ + dcl
                    gst = gstp.get()
                    for bi, (t0, n, isc) in enumerate(blocks):
                        pg = ps.get()
                        mm_fm(pg, n, wt, dcl * 128, lambda kc: uT.ap[:, kc, t0:t0 + n], [uT.us[bi]])
                        ACT(gst.ap[:, t0:t0 + n], pg.ap[:, :n], AF.Sigmoid, [pg.u], [gst.u])
                    ST(gd[dc * 128:(dc + 1) * 128, :], gst.ap, gst.u)
        if phase_end():
            return finish()
        sb.release(p2mark)
        if l == 0:
            issue_casts()

        kT = sb.alloc("kT", (HKV, T), BF16)
        LD(kT.u, kT.ap, kT_d.rearrange("(g p) t -> p g t", p=128))
        Vt = sb.alloc("Vt", (NT, DKV), BF16)
        LD(Vt.u, Vt.ap, V_d.rearrange("(i p) c -> p i c", p=128))
        Tq = S if last else T
        qhp = Pool("qh", 2, (T,), BF16)
        ptp = Pool("PT", 4, (512,), BF16)
        rdp = Pool("rden", 2, (512,), F32)
        aop = Pool("attn_o", 2, (T,), BF16)
        for h in range(H):
            g = h // 4
            qh = qhp.get()
            LD(qh.u, qh.ap[:, :Tq], qT_d[h * 128:(h + 1) * 128, 0:Tq])
            ao = aop.get()
            for (t0, n, isc) in blocks:
                if isc and last:
                    continue
                kts = list(range(NTL, NT)) if isc else list(range(NT))
                pO = ps.hold()
                pD = ps.hold()
                sbanks = []

                def emit_S(kt):
                    pS = ps.get()
                    MM(pS.ap[:, :n], kT.ap[:, g, kt * 128:(kt + 1) * 128], qh.ap[:, t0:t0 + n], True, True, [kT.u, qh.u], [pS.u])
                    sbanks.append(pS)
                emit_S(kts[0])
                for idx, kt in enumerate(kts):
                    if idx + 1 < len(kts):
                        emit_S(kts[idx + 1])
                    pS = sbanks[idx]
                    pt = ptp.get()
                    ACT(pt.ap[:, :n], pS.ap[:, :n], AF.Exp, [pS.u], [pt.u])
                    MM(pO.ap[:, :n], Vt.ap[:, kt, g * 128:(g + 1) * 128], pt.ap[:, :n], idx == 0, idx == len(kts) - 1,
                       [Vt.u, pt.u], [pO.u])
                    MM(pD.ap[:, :n], onesb.ap, pt.ap[:, :n], idx == 0, idx == len(kts) - 1, [onesb.u, pt.u], [pD.u])
                rd = rdp.get()
                P.add("dve", lambda e, rd=rd, pD=pD, n=n: e.reciprocal(out=rd.ap[:, :n], in_=pD.ap[:, :n]), [pD.u], [rd.u])
                TT("dve", ao.ap[:, t0:t0 + n], pO.ap[:, :n], rd.ap[:, :n], ALU.mult, [pO.u, rd.u], [ao.u])
                ps.unhold(pO)
                ps.unhold(pD)
            ST(attnT_d[h * 128:(h + 1) * 128, 0:Tq], ao.ap[:, :Tq], ao.u)
        if phase_end():
            return finish()
        sb.release(p1mark)

        def ln_rows(gd, bd):
            g_r = sb.alloc("lng", (D,), F32)
            b_r = sb.alloc("lnb", (D,), F32)
            LD(g_r.u, g_r.ap, gd[l:l + 1, :].partition_broadcast(128))
            LD(b_r.u, b_r.ap, bd[l:l + 1, :].partition_broadcast(128))
            return g_r, b_r
        lstat = Pool("lstat", 4, (8,), F32)
        junkp = Pool("junk", 1, (D,), BF16)

        def layer_norm_tile(r_ap, r_u, g_r, b_r):
            stt = lstat.get()
            P.add("dve", lambda e: e.tensor_reduce(out=stt.ap[:, 0:1], in_=r_ap, axis=AX.X, op=ALU.add), [r_u], [stt.u])
            TS("dve", stt.ap[:, 0:1], stt.ap[:, 0:1], -1.0 / D, None, ALU.mult, None, [stt.u], [stt.u])
            ACT(r_ap, r_ap, AF.Identity, [r_u, stt.u], [r_u], bias=stt.ap[:, 0:1])
            jk = junkp.get()
            ACT(jk.ap, r_ap, AF.Square, [r_u], [jk.u, stt.u], accum=stt.ap[:, 1:2])
            ACT(stt.ap[:, 1:2], stt.ap[:, 1:2], AF.Sqrt, [stt.u], [stt.u], bias=NORM_EPS, scale=1.0 / D)
            P.add("dve", lambda e: e.reciprocal(out=stt.ap[:, 1:2], in_=stt.ap[:, 1:2]), [stt.u], [stt.u])
            STT("dve", r_ap, r_ap, stt.ap[:, 1:2], g_r.ap, ALU.mult, ALU.mult, [r_u, stt.u, g_r.u], [r_u])
            TT("dve", r_ap, r_ap, b_r.ap, ALU.add, [r_u, b_r.u], [r_u])

        g_r, b_r = ln_rows(ln1g, ln1b)
        g1rows = []
        for v in range(1 if last else 2):
            t = sb.alloc("g1row%d" % v, (D,), F32)
            LD(t.u, t.ap, grow_d[l, v:v + 1, :].partition_broadcast(128))
            g1rows.append(t)
        wp = Pool("w5_", 3, (KC, CW), BF16)
        atp = Pool("attnT", 1, (KC, 512), BF16)
        cvtp = Pool("convT", 1, (KC, 512), BF16)
        gap = Pool("ga", 2, (NJ, 512), BF16)
        gcp = Pool("gc", 2, (NJ, 512), BF16)
        yT = sb.alloc("yT", (KC, 512), BF16)
        xr = sb.alloc("xr", (4, D), F32, nunits=4)
        t1p = Pool("t1", 2, (512,), F32)
        t2p = Pool("t2", 2, (512,), F32)
        for (t0, n, isc) in lblocks:
            v = 1 if isc else 0
            at = atp.get()
            LD(at.u, at.ap[:, :, :n], attnT_d[:, t0:t0 + n].rearrange("(kc p) t -> p kc t", p=128))
            cvt = cvtp.get()
            LD(cvt.u, cvt.ap[:, :, :n], convT_d[:, t0:t0 + n].rearrange("(kc p) t -> p kc t", p=128))
            for jt in range(n // 128):
                LD(xr.us[jt], xr.ap[:, jt, :], xsrc(l, t0 + jt * 128, 128))
            for cb in range(D // CW):
                wa = wload(wp, w_aob[l], cb * CW, CW, cast_u[("wao", l)])
                wc = wload(wp, w_cob[l], cb * CW, CW, cast_u[("wco", l)])
                ga = gap.get()
                LD(ga.u, ga.ap[:, :, :n], gaT_d[cb * CW:(cb + 1) * CW, t0:t0 + n].rearrange("(j p) t -> p j t", p=128))
                gc = gcp.get()
                LD(gc.u, gc.ap[:, :, :n], gcT_d[cb * CW:(cb + 1) * CW, t0:t0 + n].rearrange("(j p) t -> p j t", p=128))
                for dcl in range(NJ):
                    dc = cb * NJ + dcl
                    pA = ps.get()
                    mm_fm(pA, n, wa, dcl * 128, lambda kc: at.ap[:, kc, :n], [at.u])
                    pC = ps.get()
                    mm_fm(pC, n, wc, dcl * 128, lambda kc: cvt.ap[:, kc, :n], [cvt.u])
                    t1 = t1p.get()
                    t2 = t2p.get()
                    TT("dve", t1.ap[:, :n], pA.ap[:, :n], ga.ap[:, dcl, :n], ALU.mult, [pA.u, ga.u], [t1.u])
                    TT("dve", t2.ap[:, :n], pC.ap[:, :n], gc.ap[:, dcl, :n], ALU.mult, [pC.u, gc.u], [t2.u])
                    TT("dve", yT.ap[:, dc, :n], t1.ap[:, :n], t2.ap[:, :n], ALU.add, [t1.u, t2.u], [yT.u])
            for cb in range(D // CW):
                wm = wload(wp, w_mob[l], cb * CW, CW, cast_u[("wmo", l)])
                for jt in range(n // 128):
                    pM = ps.get()
                    for kc in range(KC):
                        MM(pM.ap[:, :CW], yT.ap[:, kc, jt * 128:(jt + 1) * 128], wm.ap[:, kc, :], kc == 0, kc == KC - 1,
                           [yT.u, wm.u], [pM.u])
                    t1 = t1p.get()
                    TT("dve", t1.ap[:, :CW], pM.ap[:, :CW], g1rows[v].ap[:, cb * CW:(cb + 1) * CW], ALU.mult,
                       [pM.u, g1rows[v].u], [t1.u])
                    xs_ = xr.ap[:, jt, cb * CW:(cb + 1) * CW]
                    STT("dve", xs_, xs_, float(cfg.alpha), t1.ap[:, :CW], ALU.mult, ALU.add, [xr.us[jt], t1.u], [xr.us[jt]])
            for jt in range(n // 128):
                layer_norm_tile(xr.ap[:, jt, :], xr.us[jt], g_r, b_r)
                ST(x1_d[t0 + jt * 128:t0 + (jt + 1) * 128, :], xr.ap[:, jt, :], xr.us[jt])
        if phase_end():
            return finish()
        sb.release(p1mark)

        g_r, b_r = ln_rows(ln2g, ln2b)
        lstat = Pool("lstat6", 4, (8,), F32)
        junkp = Pool("junk6", 1, (D,), BF16)
        g2row = sb.alloc("g2row", (D,), F32)
        rw = sb.alloc("rw", (KC, E), F32)
        LD(rw.u, rw.ap, rw_in[l].rearrange("(kc p) e -> p kc e", p=128))
        rwh = sb.alloc("rwh", (KC, E), BF16)
        rwl = sb.alloc("rwl", (KC, E), BF16)
        CP("dve", rwh.ap, rw.ap, [rw.u], [rwh.u])
        TT("dve", rwl.ap, rw.ap, rwh.ap, ALU.subtract, [rw.u, rwh.u], [rwl.u])
        vhi = sb.alloc("vhi", (KC, 128), BF16)
        vlo = sb.alloc("vlo", (KC, 128), BF16)
        bdp = Pool("bdr", 3, (CW,), F32)
        tyP = Pool("tmpy", 2, (CW,), F32)
        rbrow = sb.alloc("rbrow", (E,), F32)
        LD(rbrow.u, rbrow.ap, rb_in[l:l + 1, :].partition_broadcast(128))
        bupT = sb.alloc("bupT", (E * 2 * FC,), F32)
        nrow = E * 2 * FC
        for r0 in range(0, nrow, 128):
            rn = min(128, nrow - r0)
            load_T(bupT.ap[:, r0:r0 + rn], b_up[l].rearrange("e (j p) -> (e j) p", p=128)[r0:r0 + rn, :], rn, bupT.u)
        bupL = sb.alloc("bupL", (E * 2 * FC,), F32)
        TS("dve", bupL.ap, bupT.ap, 1.0, None, ALU.add, None, [bupT.u], [bupL.u])
        xpool = Pool("x6_", 1, (D,), F32)
        vT = sb.alloc("vT", (KC, 512), BF16)
        vTf = sb.alloc("vTf", (KC, 128), F32)
        acc = sb.alloc("acc", (4, D), F32, nunits=4)
        gates = sb.alloc("gates", (4, E), F32, nunits=4)
        lgp = Pool("lg", 2, (E,), F32)
        m8p = Pool("m8", 2, (8,), F32)
        mkp = Pool("mk", 2, (E,), F32)
        actp = Pool("actT", 2, (FC, 512), BF16)
        wup = Pool("wu", 2, (KC, 2, 256), BF16)
        wdp = Pool("wd", 2, (FC, CW), BF16)
        glp = Pool("glu", 2, (512,), F32)
        sgp = Pool("sig", 2, (512,), F32)
        lnp = Pool("lin", 2, (512,), F32)
        UW = min(256, F)
        NU = UW // 128
        for (t0, n, isc) in lblocks:
            v = 1 if isc else 0
            ntile = n // 128
            LD(g2row.u, g2row.ap, grow_d[l, 2 + v:3 + v, :].partition_broadcast(128))

            def router(i, col, vv):
                jt = col // 128
                pl = ps.get()
                TT("dve", vlo.ap, vTf.ap, vT.ap[:, :, col:col + 128], ALU.subtract, [vTf.u, vT.u], [vlo.u])
                trip = [(0, rwh), (0, rwl), (1, rwh)]
                for ti, (vsel, wa_) in enumerate(trip):
                    for kc in range(KC):
                        la = vT.ap[:, kc, col:col + 128] if vsel == 0 else vlo.ap[:, kc, :]
                        MM(pl.ap[:, :E], la, wa_.ap[:, kc, :], ti == 0 and kc == 0, ti == 2 and kc == KC - 1,
                           [vT.u, vlo.u, wa_.u], [pl.u])
                lg = lgp.get()
                TT("dve", lg.ap, pl.ap[:, :E], rbrow.ap, ALU.add, [pl.u, rbrow.u], [lg.u])
                m8 = m8p.get()
                P.add("dve", lambda e: e.max(out=m8.ap, in_=lg.ap), [lg.u], [m8.u])
                mk = mkp.get()
                TS("dve", mk.ap, lg.ap, m8.ap[:, 3:4], None, ALU.is_ge, None, [lg.u, m8.u], [mk.u])
                TS("dve", m8.ap[:, 0:1], m8.ap[:, 0:1], -1.0, None, ALU.mult, None, [m8.u], [m8.u])
                ACT(lg.ap, lg.ap, AF.Exp, [lg.u, m8.u], [lg.u], bias=m8.ap[:, 0:1])
                TT("dve", lg.ap, lg.ap, mk.ap, ALU.mult, [lg.u, mk.u], [lg.u])
                P.add("dve", lambda e: e.tensor_reduce(out=m8.ap[:, 1:2], in_=lg.ap, axis=AX.X, op=ALU.add), [lg.u, m8.u], [m8.u])
                P.add("dve", lambda e: e.reciprocal(out=m8.ap[:, 1:2], in_=m8.ap[:, 1:2]), [m8.u], [m8.u])
                TS("dve", gates.ap[:, jt, :], lg.ap, m8.ap[:, 1:2], None, ALU.mult, None, [lg.u, m8.u], [gates.us[jt]])
                P.add("dve", lambda e: e.memset(acc.ap[:, jt, :], 0.0), [], [acc.us[jt]])

            tiles = [(t0 // 128 + jt, jt * 128, v, vT.u) for jt in range(ntile)]
            if MOE_CUT >= 2:
                transpose_modulate(vT, lambda i: x1_d[i * 128:(i + 1) * 128, :], tiles, 3, 4, xpool, f32dst=(vTf, router))
            else:
                transpose_modulate(vT, lambda i: x1_d[i * 128:(i + 1) * 128, :], tiles, 3, 4, xpool)
                for jt in range(ntile):
                    P.add("dve", lambda e, jt=jt: e.memset(acc.ap[:, jt, :], 0.0), [], [acc.us[jt]])
            for e_ in range(E if MOE_CUT >= 3 else 0):
                actT = actp.get()
                wue = w_upb[l][e_ // EH][e_ % EH]
                for ub in range(F // UW):
                    wu = wup.get()
                    for two in range(2):
                        c0 = two * F + ub * UW
                        LD(wu.u, wu.ap[:, :, two, :UW], wue[:, c0:c0 + UW].rearrange("(kc p) f -> p kc f", p=128),
                           extra=[cast_u[("wup", l, e_ // EG)]])
                    for fl in range(NU):
                        fc = ub * NU + fl
                        pG = ps.get()
                        for kc in range(KC):
                            MM(pG.ap[:, :n], wu.ap[:, kc, 0, fl * 128:(fl + 1) * 128], vT.ap[:, kc, :n], kc == 0, kc == KC - 1,
                               [wu.u, vT.u], [pG.u])
                        pL = ps.get()
                        for kc in range(KC):
                            MM(pL.ap[:, :n], wu.ap[:, kc, 1, fl * 128:(fl + 1) * 128], vT.ap[:, kc, :n], kc == 0, kc == KC - 1,
                               [wu.u, vT.u], [pL.u])
                        bg = bupT.ap[:, e_ * 2 * FC + fc:e_ * 2 * FC + fc + 1]
                        bl = bupL.ap[:, e_ * 2 * FC + FC + fc:e_ * 2 * FC + FC + fc + 1]
                        gl = glp.get()
                        TS("dve", gl.ap[:, :n], pG.ap[:, :n], bg, SW_LIMIT, ALU.add, ALU.min, [pG.u, bupT.u], [gl.u])
                        sg = sgp.get()
                        ACT(sg.ap[:, :n], gl.ap[:, :n], AF.Sigmoid, [gl.u], [sg.u], scale=SW_ALPHA)
                        ln_ = lnp.get()
                        TS("dve", ln_.ap[:, :n], pL.ap[:, :n], bl, SW_LIMIT + 1.0, ALU.add, ALU.min, [pL.u, bupL.u], [ln_.u])
                        TT("dve", sg.ap[:, :n], sg.ap[:, :n], gl.ap[:, :n], ALU.mult, [sg.u, gl.u], [sg.u])
                        STT("dve", actT.ap[:, fc, :n], ln_.ap[:, :n], -SW_LIMIT + 1.0, sg.ap[:, :n], ALU.max, ALU.mult,
                            [sg.u, ln_.u], [actT.u])
                wde = w_dnb[l][e_].rearrange("(fc p) d -> p fc d", p=128)
                for cb in range(D // CW if MOE_CUT >= 4 else 0):
                    wd = wdp.get()
                    LD(wd.u, wd.ap, wde[:, :, cb * CW:(cb + 1) * CW], extra=[cast_u[("wdn", l, e_ // EG)]])
                    bdr = bdp.get()
                    LD(bdr.u, bdr.ap, b_dn[l, e_:e_ + 1, cb * CW:(cb + 1) * CW].partition_broadcast(128))
                    for jt in range(ntile):
                        pY = ps.get()
                        for fc in range(FC):
                            MM(pY.ap[:, :CW], actT.ap[:, fc, jt * 128:(jt + 1) * 128], wd.ap[:, fc, :], fc == 0, fc == FC - 1,
                               [actT.u, wd.u], [pY.u])
                        a_ = acc.ap[:, jt, cb * CW:(cb + 1) * CW]
                        ty = tyP.get()
                        TT("dve", ty.ap, pY.ap[:, :CW], bdr.ap, ALU.add, [pY.u, bdr.u], [ty.u])
                        STT("dve", a_, ty.ap, gates.ap[:, jt, e_:e_ + 1], a_, ALU.mult, ALU.add,
                            [ty.u, gates.us[jt], acc.us[jt]], [acc.us[jt]])
            for jt in range(ntile):
                xt = xpool.get()
                tok = t0 + jt * 128
                LD(xt.u, xt.ap, x1_d[tok:tok + 128, :])
                a_ = acc.ap[:, jt, :]
                TT("dve", a_, a_, g2row.ap, ALU.mult, [acc.us[jt], g2row.u], [acc.us[jt]])
                STT("dve", a_, xt.ap, float(cfg.alpha), a_, ALU.mult, ALU.add, [xt.u, acc.us[jt]], [acc.us[jt]])
                layer_norm_tile(a_, acc.us[jt], g_r, b_r)
                if last:
                    ST(out_d[tok:tok + 128, :], a_, acc.us[jt])
                else:
                    ST(xs_d[tok:tok + 128, :], a_, acc.us[jt])
        if phase_end():
            return finish()
    return finish()


def rope_tables(S):
    rows = S // 64
    row = np.repeat(np.arange(rows, dtype=np.int32), 64).astype(np.float32)
    col = np.tile(np.arange(64, dtype=np.int32), rows).astype(np.float32)
    inv = (np.float32(10000.0) ** (-np.arange(0, 64, 2, dtype=np.float32) / np.float32(64))).astype(np.float32)
    ang = np.concatenate([row[:, None] * inv, col[:, None] * inv], axis=-1).astype(np.float32)
    return np.cos(ang).astype(np.float32), np.sin(ang).astype(np.float32)


def make_in_maps(cfg, inputs, ncores):
    cos, sin = rope_tables(cfg.S)
    ident = np.eye(128, dtype=np.float32)
    shared = {k: np.ascontiguousarray(np.asarray(v)) for k, v in inputs.items() if k not in ("x", "c", "ctx", "c_ctx")}
    maps = []
    for b in range(ncores):
        m = dict(shared)
        m["x"] = np.ascontiguousarray(np.asarray(inputs["x"])[b])
        m["ctx"] = np.ascontiguousarray(np.asarray(inputs["ctx"])[b])
        m["c"] = np.ascontiguousarray(np.asarray(inputs["c"])[b:b + 1])
        m["c_ctx"] = np.ascontiguousarray(np.asarray(inputs["c_ctx"])[None, :])
        m["ident"] = ident
        m["cos"] = cos
        m["sin"] = sin
        maps.append(m)
    return maps


def kernel(**inputs):
    cfg = Cfg(2048, 2048, 256, 32)
    nc = build(cfg)
    maps = make_in_maps(cfg, inputs, 8)
    res = run_bass_kernel_spmd(nc, maps, core_ids=list(range(8)))
    return np.stack([np.asarray(r["out"]) for r in res.results], axis=0).astype(np.float32)
```
